# Optimizing a Trainium2 kernel written in Bass

```python
import math
import jax
import jax.numpy as jnp
from jax import lax
import numpy as np

D_MODEL = 4096
BATCH = 2
SEQ = 4096
DEPTH = 1

CTX_LEN = 256
GRID_W = 64
N_MOD = 6
EPS = 1e-6

N_Q_HEADS = 32
N_KV_HEADS = 8
HEAD_DIM = 128
GQA_GROUP = N_Q_HEADS // N_KV_HEADS
Q_BLOCK = 128
ROPE_THETA = 10000.0
ROPE_AXIS_FREQS = HEAD_DIM // 4

SSD_INNER = D_MODEL
SSD_HEAD_DIM = 64
SSD_HEADS = SSD_INNER // SSD_HEAD_DIM
SSD_GROUPS = 8
SSD_HEADS_PER_GROUP = SSD_HEADS // SSD_GROUPS
SSD_STATE = 128
CONV_W = 5
CHUNK = 128

ATTN_Q_DIM = N_Q_HEADS * HEAD_DIM
ATTN_KV_DIM = N_KV_HEADS * HEAD_DIM
BC_DIM = SSD_GROUPS * SSD_STATE
XBC_DIM = SSD_INNER + 2 * BC_DIM
CTX_COLS = 2 * ATTN_KV_DIM + XBC_DIM + 2 * SSD_HEADS
IN_COLS = CTX_COLS + ATTN_Q_DIM + 2 * D_MODEL + SSD_INNER

N_GROUPS = 4
EXPERTS_PER_GROUP = 8
N_EXPERTS = N_GROUPS * EXPERTS_PER_GROUP
TOP_K = 2
D_EXPERT = D_MODEL // 4
MOE_BLOCK = 128

kernel_name = 'hybrid_gqa_ssd_hmoe_dit_block'


def rms_norm(x, w):
    xf = x.astype(jnp.float32)
    xf = xf * lax.rsqrt(jnp.mean(xf * xf, axis=-1, keepdims=True) + EPS)
    return xf.astype(x.dtype) * w


def modulate(h, shift, scale):
    return h * (1.0 + scale) + shift


def adaln(cond, p):
    mod = jax.nn.silu(cond) @ p['w_ada'] + p['b_ada']
    return jnp.split(mod, N_MOD, axis=-1)


def rope_tables(seq_len):
    rows = seq_len // GRID_W
    row_pos = jnp.repeat(jnp.arange(rows, dtype=jnp.float32), GRID_W)
    col_pos = (jnp.arange(seq_len) % GRID_W).astype(jnp.float32)
    inv_freq = ROPE_THETA ** (-jnp.arange(ROPE_AXIS_FREQS, dtype=jnp.float32) / ROPE_AXIS_FREQS)
    ang_r = row_pos[:, None] * inv_freq
    ang_c = col_pos[:, None] * inv_freq
    return (jnp.cos(ang_r), jnp.sin(ang_r), jnp.cos(ang_c), jnp.sin(ang_c))


def rotate_pairs(x, cos, sin):
    f = x.shape[-1] // 2
    x1, x2 = x[..., :f], x[..., f:]
    c, s = cos[:, None, :], sin[:, None, :]
    return jnp.concatenate([x1 * c - x2 * s, x2 * c + x1 * s], axis=-1)


def apply_rope_2d(x, rope):
    cos_r, sin_r, cos_c, sin_c = rope
    half = HEAD_DIM // 2
    xr = rotate_pairs(x[..., :half], cos_r, sin_r)
    xc = rotate_pairs(x[..., half:], cos_c, sin_c)
    return jnp.concatenate([xr, xc], axis=-1).astype(x.dtype)


def centred_dwconv(x, w, b):
    pad = CONV_W // 2
    length = x.shape[1]
    xp = jnp.pad(x, ((0, 0), (pad, pad), (0, 0)))
    out = xp[:, 0:length] * w[0]
    for j in range(1, CONV_W):
        out = out + xp[:, j:j + length] * w[j]
    return out + b


def kv_ssd_inputs(part, p):
    b, l = part.shape[:2]
    o1, o2 = ATTN_KV_DIM, 2 * ATTN_KV_DIM
    o3 = o2 + XBC_DIM
    k = rms_norm(part[..., :o1].reshape(b, l, N_KV_HEADS, HEAD_DIM), p['k_norm_w'])
    v = part[..., o1:o2].reshape(b, l, N_KV_HEADS, HEAD_DIM)
    xbc = jax.nn.silu(centred_dwconv(part[..., o2:o3], p['conv_w'], p['conv_b']))
    xs = xbc[..., :SSD_INNER].reshape(b, l, SSD_GROUPS, SSD_HEADS_PER_GROUP, SSD_HEAD_DIM)
    bm = xbc[..., SSD_INNER:SSD_INNER + BC_DIM].reshape(b, l, SSD_GROUPS, SSD_STATE)
    cm = xbc[..., SSD_INNER + BC_DIM:].reshape(b, l, SSD_GROUPS, SSD_STATE)
    dt = part[..., o3:].astype(jnp.float32)
    dtf = jax.nn.softplus(dt[..., :SSD_HEADS] + p['dt_bias_f'].astype(jnp.float32))
    dtb = jax.nn.softplus(dt[..., SSD_HEADS:] + p['dt_bias_b'].astype(jnp.float32))
    dtf = dtf.reshape(b, l, SSD_GROUPS, SSD_HEADS_PER_GROUP)
    dtb = dtb.reshape(b, l, SSD_GROUPS, SSD_HEADS_PER_GROUP)
    return k, v, xs, bm, cm, dtf, dtb


def q_gate_z(proj):
    o1 = CTX_COLS
    o2 = o1 + ATTN_Q_DIM
    o3 = o2 + 2 * D_MODEL
    return proj[..., o1:o2], proj[..., o2:o3], proj[..., o3:]


def block_attention(q, k, v):
    b, s = q.shape[:2]
    n_blk = s // Q_BLOCK
    qb = q.reshape(b, n_blk, Q_BLOCK, N_KV_HEADS, GQA_GROUP, HEAD_DIM).transpose(1, 0, 2, 3, 4, 5)
    scale = HEAD_DIM ** -0.5

    def one_block(qi):
        sc = jnp.einsum('bqkgd,blkd->bkgql', qi, k).astype(jnp.float32) * scale
        pr = jax.nn.softmax(sc, axis=-1).astype(v.dtype)
        return jnp.einsum('bkgql,blkd->bqkgd', pr, v)

    o = lax.map(one_block, qb)
    return o.transpose(1, 0, 2, 3, 4, 5).reshape(b, s, ATTN_Q_DIM)


def ssd_chunked(xs, dt, a, bm, cm, h0):
    b, length = xs.shape[:2]
    nc = length // CHUNK

    def to_chunks(t):
        return jnp.moveaxis(t.reshape((b, nc, CHUNK) + t.shape[2:]), 1, 0)

    lower = jnp.tril(jnp.ones((CHUNK, CHUNK), dtype=bool))[None, :, :, None, None]

    def step(h, inp):
        xc, dtc, bc, cc = inp
        acum = jnp.cumsum(dtc * a, axis=1)
        seg = acum[:, :, None] - acum[:, None, :]
        decay = jnp.exp(jnp.where(lower, seg, -jnp.inf))
        cb = jnp.einsum('bign,bjgn->bijg', cc, bc)
        wmat = cb[..., None] * decay * dtc[:, None]
        y = jnp.einsum('bijgr,bjgrp->bigrp', wmat, xc)
        y = y + jnp.einsum('bign,bgrpn->bigrp', cc, h) * jnp.exp(acum)[..., None]
        to_end = jnp.exp(acum[:, -1:] - acum) * dtc
        h_new = h * jnp.exp(acum[:, -1])[..., None, None] + jnp.einsum('bjgn,bjgr,bjgrp->bgrpn', bc, to_end, xc)
        return h_new, y

    h_fin, ys = lax.scan(step, h0, (to_chunks(xs), to_chunks(dt), to_chunks(bm), to_chunks(cm)))
    y = jnp.moveaxis(ys, 0, 1).reshape(xs.shape).astype(xs.dtype)
    return y, h_fin


def ssd_final_state(xs, dt, a, bm):
    acum = jnp.cumsum(dt * a, axis=1)
    to_end = jnp.exp(acum[:, -1:] - acum) * dt
    return jnp.einsum('blgn,blgr,blgrp->bgrpn', bm, to_end, xs)


def flip_seq(t):
    return jnp.flip(t, axis=1)


def merge_branches(attn, y_ssd, xs, z, gates, p):
    b, l = attn.shape[:2]
    y = (y_ssd + p['d_skip'].reshape(SSD_GROUPS, SSD_HEADS_PER_GROUP, 1) * xs).reshape(b, l, SSD_INNER)
    y = rms_norm(y * jax.nn.silu(z), p['ssd_norm_w'])
    g = jax.nn.sigmoid(gates.astype(jnp.float32)).astype(attn.dtype)
    merged = g[..., :D_MODEL] * (attn @ p['w_attn_proj']) + g[..., D_MODEL:] * (y @ p['w_ssd_proj'])
    return merged @ p['w_out']


def hier_moe(h, p):
    t = h.shape[0]
    g_logits = (h @ p['w_router_group']).astype(jnp.float32) + p['b_router_group'].astype(jnp.float32)
    g_prob, g_idx = lax.top_k(jax.nn.softmax(g_logits, axis=-1), 1)
    e_logits = (h @ p['w_router_expert']).astype(jnp.float32) + p['b_router_expert'].astype(jnp.float32)
    e_logits = e_logits.reshape(t, N_GROUPS, EXPERTS_PER_GROUP)
    e_logits = jnp.take_along_axis(e_logits, g_idx[:, :, None], axis=1)[:, 0]
    e_prob, e_idx = lax.top_k(jax.nn.softmax(e_logits, axis=-1), TOP_K)
    weights = g_prob * e_prob / jnp.sum(e_prob, axis=-1, keepdims=True)
    expert_id = g_idx * EXPERTS_PER_GROUP + e_idx

    n_assign = t * TOP_K
    flat_e = expert_id.reshape(-1)
    flat_t = jnp.repeat(jnp.arange(t, dtype=jnp.int32), TOP_K)
    flat_w = weights.reshape(-1)
    order = jnp.argsort(flat_e)
    se, st, sw = flat_e[order], flat_t[order], flat_w[order]
    counts = jnp.bincount(flat_e, length=N_EXPERTS)
    starts = jnp.cumsum(counts) - counts
    padded = (counts + MOE_BLOCK - 1) // MOE_BLOCK * MOE_BLOCK
    pends = jnp.cumsum(padded)
    pstarts = pends - padded
    dest = pstarts[se] + jnp.arange(n_assign) - starts[se]
    n_rows = (-(-n_assign // MOE_BLOCK) + N_EXPERTS) * MOE_BLOCK
    n_blocks = n_rows // MOE_BLOCK
    row_tok = jnp.zeros((n_rows,), jnp.int32).at[dest].set(st)
    row_w = jnp.zeros((n_rows,), h.dtype).at[dest].set(sw.astype(h.dtype))
    blk_e = jnp.minimum(jnp.searchsorted(pends, jnp.arange(n_blocks) * MOE_BLOCK, side='right'), N_EXPERTS - 1)
    xb = h[row_tok].reshape(n_blocks, MOE_BLOCK, h.shape[1])

    def expert_block(args):
        xi, e = args
        a = xi @ p['w_exp_gate'][e]
        u = xi @ p['w_exp_up'][e]
        return (jax.nn.silu(a) * u) @ p['w_exp_down'][e]

    yb = lax.map(expert_block, (xb, blk_e)).reshape(n_rows, h.shape[1])
    return jnp.zeros_like(h).at[row_tok].add(yb * row_w[:, None])


def trunk_layer(x, ctx, c, c_ctx, p, rope, update_ctx):
    b, s, d = x.shape
    a_f = -jnp.exp(p['a_log_f'].astype(jnp.float32)).reshape(SSD_GROUPS, SSD_HEADS_PER_GROUP)
    a_b = -jnp.exp(p['a_log_b'].astype(jnp.float32)).reshape(SSD_GROUPS, SSD_HEADS_PER_GROUP)
    sh_m, sc_m, gt_m, sh_f, sc_f, gt_f = [m[:, None, :] for m in adaln(c, p)]
    csh_m, csc_m, cgt_m, csh_f, csc_f, cgt_f = adaln(c_ctx, p)

    hc = modulate(rms_norm(ctx, p['norm_mix_w']), csh_m, csc_m)
    w_in_ctx = p['w_in'] if update_ctx else p['w_in'][:, :CTX_COLS]
    proj_c = hc @ w_in_ctx
    kc, vc, xs_c, b_c, c_c, dtf_c, dtb_c = kv_ssd_inputs(proj_c[..., :CTX_COLS], p)
    if update_ctx:
        q_c, gates_c, z_c = q_gate_z(proj_c)
        q_c = rms_norm(q_c.reshape(b, CTX_LEN, N_Q_HEADS, HEAD_DIM), p['q_norm_w'])
        attn_c = block_attention(q_c, kc, vc)
        h0 = jnp.zeros((b, SSD_GROUPS, SSD_HEADS_PER_GROUP, SSD_HEAD_DIM, SSD_STATE), jnp.float32)
        yf_c, hf_c = ssd_chunked(xs_c, dtf_c, a_f, b_c, c_c, h0)
        yb_c, hb_c = ssd_chunked(flip_seq(xs_c), flip_seq(dtb_c), a_b, flip_seq(b_c), flip_seq(c_c), h0)
        ctx_next = ctx + cgt_m * merge_branches(attn_c, yf_c + flip_seq(yb_c), xs_c, z_c, gates_c, p)
        hc2 = modulate(rms_norm(ctx_next, p['norm_ffn_w']), csh_f, csc_f)
        ctx_next = ctx_next + cgt_f * hier_moe(hc2.reshape(-1, d), p).reshape(ctx.shape)
    else:
        hf_c = ssd_final_state(xs_c, dtf_c, a_f, b_c)
        hb_c = ssd_final_state(flip_seq(xs_c), flip_seq(dtb_c), a_b, flip_seq(b_c))
        ctx_next = ctx

    hx = modulate(rms_norm(x, p['norm_mix_w']), sh_m, sc_m)
    proj = hx @ p['w_in']
    kx, vx, xs, bm, cm, dtf, dtb = kv_ssd_inputs(proj[..., :CTX_COLS], p)
    q, gates, z = q_gate_z(proj)
    q = apply_rope_2d(rms_norm(q.reshape(b, s, N_Q_HEADS, HEAD_DIM), p['q_norm_w']), rope)
    kx = apply_rope_2d(kx, rope)
    attn = block_attention(q, jnp.concatenate([kx, kc], axis=1), jnp.concatenate([vx, vc], axis=1))
    yf, _ = ssd_chunked(xs, dtf, a_f, bm, cm, hf_c)
    yb, _ = ssd_chunked(flip_seq(xs), flip_seq(dtb), a_b, flip_seq(bm), flip_seq(cm), hb_c)
    x = x + gt_m * merge_branches(attn, yf + flip_seq(yb), xs, z, gates, p)

    hx2 = modulate(rms_norm(x, p['norm_ffn_w']), sh_f, sc_f)
    x = x + gt_f * hier_moe(hx2.reshape(-1, d), p).reshape(b, s, d)
    return x, ctx_next


def setup_inputs(seed: int = 0) -> dict:
    key = jax.random.key(seed)
    ks = jax.random.split(key, 32)
    f32 = jnp.float32
    L = DEPTH

    def nrm(k, shape, scale):
        return jax.random.normal(k, shape, f32) * scale

    def gain(k, shape):
        return 1.0 + 0.01 * jax.random.normal(k, shape, f32)

    def dt_bias(k):
        dt0 = jnp.exp(jax.random.uniform(k, (L, SSD_HEADS), f32, math.log(1e-3), math.log(1e-1)))
        return dt0 + jnp.log(-jnp.expm1(-dt0))

    dinv = D_MODEL ** -0.5
    return {
        'x': nrm(ks[0], (BATCH, SEQ, D_MODEL), 1.0),
        'c': nrm(ks[1], (BATCH, D_MODEL), 1.0),
        'ctx': nrm(ks[2], (BATCH, CTX_LEN, D_MODEL), 1.0),
        'c_ctx': nrm(ks[3], (D_MODEL,), 1.0),
        'w_ada': nrm(ks[4], (L, D_MODEL, N_MOD * D_MODEL), 0.5 * dinv),
        'b_ada': nrm(ks[5], (L, N_MOD * D_MODEL), 0.01),
        'norm_mix_w': gain(ks[6], (L, D_MODEL)),
        'norm_ffn_w': gain(ks[7], (L, D_MODEL)),
        'w_in': nrm(ks[8], (L, D_MODEL, IN_COLS), dinv),
        'q_norm_w': gain(ks[9], (L, HEAD_DIM)),
        'k_norm_w': gain(ks[10], (L, HEAD_DIM)),
        'conv_w': nrm(ks[11], (L, CONV_W, XBC_DIM), CONV_W ** -0.5),
        'conv_b': nrm(ks[12], (L, XBC_DIM), 0.01),
        'a_log_f': jnp.log(jax.random.uniform(ks[13], (L, SSD_HEADS), f32, 1.0, 16.0)),
        'a_log_b': jnp.log(jax.random.uniform(ks[14], (L, SSD_HEADS), f32, 1.0, 16.0)),
        'dt_bias_f': dt_bias(ks[15]),
        'dt_bias_b': dt_bias(ks[16]),
        'd_skip': gain(ks[17], (L, SSD_HEADS)),
        'ssd_norm_w': gain(ks[18], (L, SSD_INNER)),
        'w_attn_proj': nrm(ks[19], (L, ATTN_Q_DIM, D_MODEL), ATTN_Q_DIM ** -0.5),
        'w_ssd_proj': nrm(ks[20], (L, SSD_INNER, D_MODEL), SSD_INNER ** -0.5),
        'w_out': nrm(ks[21], (L, D_MODEL, D_MODEL), dinv),
        'w_router_group': nrm(ks[22], (L, D_MODEL, N_GROUPS), dinv),
        'b_router_group': nrm(ks[23], (L, N_GROUPS), 0.01),
        'w_router_expert': nrm(ks[24], (L, D_MODEL, N_EXPERTS), dinv),
        'b_router_expert': nrm(ks[25], (L, N_EXPERTS), 0.01),
        'w_exp_gate': nrm(ks[26], (L, N_EXPERTS, D_MODEL, D_EXPERT), dinv),
        'w_exp_up': nrm(ks[27], (L, N_EXPERTS, D_MODEL, D_EXPERT), dinv),
        'w_exp_down': nrm(ks[28], (L, N_EXPERTS, D_EXPERT, D_MODEL), D_EXPERT ** -0.5),
    }


def reference(x, c, ctx, c_ctx, w_ada, b_ada, norm_mix_w, norm_ffn_w, w_in, q_norm_w, k_norm_w,
              conv_w, conv_b, a_log_f, a_log_b, dt_bias_f, dt_bias_b, d_skip, ssd_norm_w,
              w_attn_proj, w_ssd_proj, w_out, w_router_group, b_router_group, w_router_expert,
              b_router_expert, w_exp_gate, w_exp_up, w_exp_down):
    rope = rope_tables(x.shape[1])
    for i in range(DEPTH):
        p = {
            'w_ada': w_ada[i], 'b_ada': b_ada[i],
            'norm_mix_w': norm_mix_w[i], 'norm_ffn_w': norm_ffn_w[i],
            'w_in': w_in[i], 'q_norm_w': q_norm_w[i], 'k_norm_w': k_norm_w[i],
            'conv_w': conv_w[i], 'conv_b': conv_b[i],
            'a_log_f': a_log_f[i], 'a_log_b': a_log_b[i],
            'dt_bias_f': dt_bias_f[i], 'dt_bias_b': dt_bias_b[i],
            'd_skip': d_skip[i], 'ssd_norm_w': ssd_norm_w[i],
            'w_attn_proj': w_attn_proj[i], 'w_ssd_proj': w_ssd_proj[i], 'w_out': w_out[i],
            'w_router_group': w_router_group[i], 'b_router_group': b_router_group[i],
            'w_router_expert': w_router_expert[i], 'b_router_expert': b_router_expert[i],
            'w_exp_gate': w_exp_gate[i], 'w_exp_up': w_exp_up[i], 'w_exp_down': w_exp_down[i],
        }
        x, ctx = trunk_layer(x, ctx, c, c_ctx, p, rope, i < DEPTH - 1)
    return x
```

```python
import numpy as np
from contextlib import ExitStack
import concourse.bass as bass
import concourse.mybir as mybir
from concourse.bass_utils import run_bass_kernel_spmd

F32 = mybir.dt.float32
BF16 = mybir.dt.bfloat16
I32 = mybir.dt.int32
AF = mybir.ActivationFunctionType
ALU = mybir.AluOpType
AX = mybir.AxisListType
EPS = 1e-6
ROPE_THETA = 10000.0


class Cfg:
    def __init__(self, D=4096, S=4096, CTX=256, HQ=4, DE=1024, CAP=1024, GW=64):
        self.B = 2
        self.D, self.S, self.CTX, self.HQ, self.DE, self.CAP, self.GW = D, S, CTX, HQ, DE, CAP, GW
        self.KC = D // 128
        self.DS = D // 8
        self.CD = self.DS // 128
        self.HPG = D // 64 // 8
        self.QF = HQ * 128
        self.QC = HQ
        self.EC = DE // 128
        self.TPC = 2 * S // 8
        self.CT = 2 * CTX // 8
        self.TW = self.TPC + self.CT
        self.NT = 2 * S
        self.NTT = self.NT // 128
        self.L = S + CTX
        self.NCC = self.CD + 2
        DS, HPG = self.DS, self.HPG
        o = 0
        self.oq = o; o += self.QF
        self.ok = o; o += 128
        self.ov = o; o += 128
        self.ox = o; o += DS
        self.oB = o; o += 128
        self.oC = o; o += 128
        self.odt = o; o += 2 * HPG
        self.oga = o; o += DS
        self.ogs = o; o += DS
        self.oz = o; o += DS
        self.NC1 = o
        self.NBLK = CAP // 128


class FW:
    NDS = 40

    def __init__(self, nc, es):
        self.nc = nc
        self.E = dict(pe=nc.tensor, act=nc.scalar, dve=nc.vector, pool=nc.gpsimd, sp=nc.sync)
        self.sem = {}
        for e in self.E:
            self.sem[('c', e)] = es.enter_context(nc.semaphore("s_" + e))
        self.cnt = {e: 0 for e in self.E}
        for i in range(self.NDS):
            self.sem[('d', i)] = es.enter_context(nc.semaphore("d%d" % i))
        self.dval = [0] * self.NDS
        self.dnext = 0
        self.dnext_sw = 0
        self.NSP = 28
        self.sem[('cc', 0)] = es.enter_context(nc.semaphore("ccs"))
        self.ccval = 0
        self.waited = {}
        self.lastw = {}
        self.readers = {}
        self.nins = 0

    def _pick(self, q):
        if q == 'pool':
            i = self.NSP + self.dnext_sw
            self.dnext_sw = (self.dnext_sw + 1) % (self.NDS - self.NSP)
        else:
            i = self.dnext
            self.dnext = (self.dnext + 1) % self.NSP
        return i

    def _wait(self, e, ev):
        sk, val = ev
        if sk == ('c', 'pe') and e == 'pe':
            return
        if self.waited.get((e, sk), 0) >= val:
            return
        self.E[e].wait_ge(self.sem[sk], val)
        self.waited[(e, sk)] = val

    def _deps(self, e, r, w):
        for k in r:
            ev = self.lastw.get(k)
            if ev is not None:
                self._wait(e, ev)
        for k in w:
            ev = self.lastw.get(k)
            if ev is not None:
                self._wait(e, ev)
            for ev in self.readers.get(k, {}).values():
                self._wait(e, ev)

    def _record(self, ev, r, w):
        for k in r:
            self.readers.setdefault(k, {})[ev[0]] = ev
        for k in w:
            self.lastw[k] = ev
            self.readers[k] = {}

    def op(self, e, fn, r=(), w=()):
        self._deps(e, r, w)
        ins = fn(self.E[e])
        self.cnt[e] += 1
        ins.then_inc(self.sem[('c', e)], 1)
        self._record((('c', e), self.cnt[e]), r, w)
        self.nins += 1

    def dma(self, q, out, in_, r=(), w=(), **kw):
        self._deps(q, r, w)
        i = self._pick(q)
        if self.dval[i] > 0:
            self._wait(q, (('d', i), self.dval[i]))
        ins = self.E[q].dma_start(out=out, in_=in_, **kw)
        self.dval[i] += 16
        ins.then_inc(self.sem[('d', i)], 16)
        self._record((('d', i), self.dval[i]), r, w)
        self.nins += 1

    def idma(self, r=(), w=(), **kw):
        q = 'pool'
        self._deps(q, r, w)
        i = self._pick(q)
        if self.dval[i] > 0:
            self._wait(q, (('d', i), self.dval[i]))
        ins = self.E[q].indirect_dma_start(**kw)
        self.dval[i] += 16
        ins.then_inc(self.sem[('d', i)], 16)
        self._record((('d', i), self.dval[i]), r, w)

    def allgather(self, in_ap, out_ap, r=(), w=(), inc=1):
        self._deps('pool', r, w)
        ins = self.E['pool'].collective_compute(
            "AllGather", ALU.bypass, replica_groups=[list(range(8))], ins=[in_ap], outs=[out_ap])
        self.ccval += inc
        ins.then_inc(self.sem[('cc', 0)], inc)
        self._record((('cc', 0), self.ccval), r, w)

    def barrier(self):
        evs = [(('c', f), self.cnt[f]) for f in self.E if self.cnt[f] > 0]
        evs += [(('d', i), self.dval[i]) for i in range(self.NDS) if self.dval[i] > 0]
        for e in self.E:
            for ev in evs:
                if ev[0] == ('c', e):
                    continue
                self._wait(e, ev)
        for e in ('act', 'dve', 'pool'):
            if self.cnt[e]:
                self._wait(e, (('c', e), self.cnt[e]))
        keep = {k: ev for k, ev in self.lastw.items() if ev[0] == ('cc', 0)}
        self.lastw = keep
        self.readers = {}


def _ceil(a, b):
    return (a + b - 1) // b


def build(cfg, debug=()):
    c = cfg
    D, S, CTX, KC, DS, CD, HPG, HQ, QF = c.D, c.S, c.CTX, c.KC, c.DS, c.CD, c.HPG, c.HQ, c.QF
    NT, NTT, L, TPC, CT, TW, NCC = c.NT, c.NTT, c.L, c.TPC, c.CT, c.TW, c.NCC
    DE, EC, CAP, NBLK = c.DE, c.EC, c.CAP, c.NBLK
    nc = bass.Bass("TRN2", target_bir_lowering=False)
    T = {}

    def inp(name, shape, dt=F32):
        T[name] = nc.dram_tensor(name, list(shape), dt, kind="ExternalInput").ap()

    dbgcopy = []

    def scr(name, shape, dt=F32, coll=False):
        kind = {}
        if (name in debug) and not coll:
            kind = dict(kind="ExternalOutput")
        T[name] = nc.dram_tensor(name, list(shape), dt, **kind).ap()
        if (name in debug) and coll:
            T["dbg_" + name] = nc.dram_tensor("dbg_" + name, list(shape), dt, kind="ExternalOutput").ap()
            dbgcopy.append(name)

    inp("xtok", [TPC, D]); inp("ctok", [CT, D]); inp("xcol", [NT, DS]); inp("cvec", [3, D])
    inp("wada", [D, 6 * DS]); inp("bada", [1, 6 * DS]); inp("nmw", [128, KC]); inp("nfw", [1, DS])
    inp("w1", [D, c.NC1]); inp("qknw", [128, 2]); inp("convw", [128, NCC * 6]); inp("ssdv", [1, 5 * HPG])
    inp("snw", [1, DS]); inp("wap", [8 * QF, DS]); inp("wsp", [D, DS]); inp("wout", [D, DS])
    inp("wr", [DS, 36]); inp("br", [1, 36]); inp("weg", [4, D, DE]); inp("weu", [4, D, DE]); inp("wed", [4, DE, D])
    inp("cmat", [128, 6 * 128]); inp("ropec", [128, S]); inp("ropes", [128, S]); inp("rk", [128, 40]); inp("rkb", [128, 64])
    scr("modloc", [3, 6 * DS], coll=True); scr("modall", [24, 6 * DS], coll=True)
    scr("hxT_loc", [D, TW], BF16, coll=True); scr("hxT_all", [8 * D, TW], BF16, coll=True)
    scr("qT", [HQ * 128, NT], BF16); scr("kT", [128, 2 * L], BF16); scr("vv", [2 * L, 128], BF16)
    scr("xbc", [NCC * 128, 2 * L]); scr("dtr", [2 * L, 2 * HPG])
    scr("gaT", [DS, NT], BF16); scr("gsT", [DS, NT], BF16); scr("szt", [NT, DS])
    scr("ATloc", [QF, NT], BF16, coll=True); scr("ATall", [8 * QF, NT], BF16, coll=True)
    scr("YNloc", [DS, NT], BF16, coll=True); scr("YNall", [8 * DS, NT], BF16, coll=True)
    scr("ygs", [NT, DS]); scr("ss1loc", [128, NTT], coll=True); scr("ss1all", [8 * 128, NTT], coll=True)
    scr("mT_loc", [DS, NT], BF16, coll=True); scr("mT_all", [D, NT], BF16, coll=True)
    scr("x1", [NT, DS]); scr("ss2loc", [128, NTT], coll=True); scr("ss2all", [8 * 128, NTT], coll=True)
    scr("hx2_loc", [NT, DS], BF16, coll=True); scr("hx2_all", [8 * NT, DS], BF16, coll=True)
    scr("lg_loc", [NT, 36], coll=True); scr("lg_all", [8 * NT, 36], coll=True)
    scr("Xsel", [4 * CAP, D], BF16)
    for ab in "ab":
        scr("Yloc_" + ab, [8 * 2 * CAP, DS], BF16, coll=True); scr("Yall_" + ab, [64 * 2 * CAP, DS], BF16, coll=True)
    T["out"] = nc.dram_tensor("out", [NT, DS], F32, kind="ExternalOutput").ap()
    for name in debug:
        if name.startswith("dbg_"):
            pass

    with ExitStack() as es:
        fw = FW(nc, es)
        G = {}
        for n_, dt_ in (("cm_f", F32), ("cm_b", BF16)):
            G[n_] = es.enter_context(nc.sbuf_tensor(n_, [128, 6 * 128], dt_))
        G["A1"] = es.enter_context(nc.sbuf_tensor("A1", [128, 3, KC], F32))
        G["B1"] = es.enter_context(nc.sbuf_tensor("B1", [128, 3, KC], F32))
        G["rk"] = es.enter_context(nc.sbuf_tensor("rk_sb", [128, 40], F32))
        G["rkb"] = es.enter_context(nc.sbuf_tensor("rkb_sb", [128, 64], F32))
        es.enter_context(nc.Block())
        fw.dma('sp', G["cm_f"][:], T["cmat"][:, :], w=["cm_f"])
        fw.dma('sp', G["rk"][:], T["rk"][:, :], w=["rk"])
        fw.dma('sp', G["rkb"][:], T["rkb"][:, :], w=["rkb"])
        fw.op('dve', lambda e: e.tensor_copy(out=G["cm_b"][:], in_=G["cm_f"][:]), r=["cm_f"], w=["cm_b"])
        fw.barrier()
        for i, nm in enumerate(["ident", "triinc", "tridec", "ones", "perm", "triexc"]):
            G[nm + "_f"] = G["cm_f"][:, i * 128:(i + 1) * 128]
            G[nm + "_b"] = G["cm_b"][:, i * 128:(i + 1) * 128]

        import os
        stop = os.environ.get("K_STOP", "99")
        for i_, st_ in enumerate([stage0, stage1, stage2, stage3, stage4, stage5, stage6, stage7, stage8]):
            if i_ > int(stop[0:2].rstrip("ABC") or 99):
                break
            st_(fw, c, T, G)
        fw.barrier()
        for name in dbgcopy:
            fw.dma('sp', T["dbg_" + name], T[name], r=[name], w=["dbg_" + name])
        fw.barrier()
        print("instructions emitted:", fw.nins)
    return nc


def _phase(nc):
    ph = ExitStack()
    sb = lambda n, s, d=F32: ph.enter_context(nc.sbuf_tensor(n, list(s), d))
    ps = lambda n, s, d=F32: ph.enter_context(nc.psum_tensor(n, list(s), d))
    return ph, sb, ps


def stage0(fw, c, T, G):
    nc = fw.nc
    D, KC, DS, CD = c.D, c.KC, c.DS, c.CD
    N6 = 6 * DS
    nb = _ceil(N6, 512)
    ph, sb, ps = _phase(nc)
    with ph:
        cv = sb("s0cv", [3, D]); sc = sb("s0sc", [3, D]); scT = sb("s0scT", [128, KC * 3])
        bb = sb("s0bb", [3, N6]); ml = sb("s0ml", [3, N6])
        wt = [sb("s0wt%d" % i, [128, N6]) for i in range(2)]
        pT = ps("s0pT", [128, 512])
        pm = [ps("s0pm%d" % j, [128, 512]) for j in range(nb)]
        fw.dma('sp', cv[:], T["cvec"][:, :], w=["cv"])
        fw.dma('sp', bb[:], T["bada"][0:1, :].partition_broadcast(3), w=["bb"])
        fw.op('act', lambda e: e.activation(out=sc[:], in_=cv[:], func=AF.Silu), r=["cv"], w=["sc"])
        for kc in range(KC):
            fw.op('pe', lambda e: e.transpose(out=pT[:, kc * 3:(kc + 1) * 3], in_=sc[:3, kc * 128:(kc + 1) * 128],
                                              identity=G["ident_f"][:3, :3]), r=["sc"], w=["pT"])
        fw.op('dve', lambda e: e.tensor_copy(out=scT[:], in_=pT[:, :KC * 3]), r=["pT"], w=["scT"])
        for kc in range(KC):
            b = kc % 2
            fw.dma('sp', wt[b][:], T["wada"][kc * 128:(kc + 1) * 128, :], w=[("wt", b)])
            for j in range(nb):
                n0, n1 = j * 512, min(N6, j * 512 + 512)
                fw.op('pe', lambda e: e.matmul(pm[j][:3, :n1 - n0], lhsT=scT[:, kc * 3:(kc + 1) * 3], rhs=wt[b][:, n0:n1],
                                               start=(kc == 0), stop=(kc == KC - 1)), r=[("wt", b), "scT"], w=[("pm", j)])
        for j in range(nb):
            n0, n1 = j * 512, min(N6, j * 512 + 512)
            fw.op('dve', lambda e: e.tensor_tensor(out=ml[:, n0:n1], in0=pm[j][:3, :n1 - n0], in1=bb[:, n0:n1], op=ALU.add),
                  r=[("pm", j), "bb"], w=["ml"])
        fw.dma('sp', T["modloc"][:, :], ml[:], r=["ml"], w=["modloc"])
        fw.allgather(T["modloc"], T["modall"], r=["modloc"], w=["modall"])
        m24 = sb("s0m24", [24, N6]); mt = sb("s0mt", [128, 6, CD * 24]); nm = sb("s0nm", [128, KC])
        pT2 = ps("s0pT2", [128, 512])
        fw.dma('sp', m24[:], T["modall"][:, :], r=["modall"], w=["m24"])
        fw.dma('sp', nm[:], T["nmw"][:, :], w=["nm"])
        for j in range(2):
            for cc in range(CD):
                fw.op('pe', lambda e: e.transpose(out=pT2[:, j * 128 + cc * 24: j * 128 + cc * 24 + 24],
                                                  in_=m24[:24, j * DS + cc * 128: j * DS + cc * 128 + 128],
                                                  identity=G["ident_f"][:24, :24]), r=["m24"], w=["pT2"])
        fw.op('dve', lambda e: e.tensor_copy(out=mt[:, 0:2, :], in_=pT2[:, 0:256].rearrange("p (j x) -> p j x", j=2)[:, :, :CD * 24]),
              r=["pT2"], w=["mt"])
        for i in range(3):
            def view(j):
                return mt[:, j, :].rearrange("p (cc g i) -> p g cc i", cc=CD, g=8, i=3)[:, :, :, i]
            a_out = G["A1"][:, i, :].rearrange("p (g cc) -> p g cc", g=8)
            b_out = G["B1"][:, i, :].rearrange("p (g cc) -> p g cc", g=8)
            nmv = nm[:, :].rearrange("p (g cc) -> p g cc", g=8)
            fw.op('dve', lambda e: e.tensor_scalar(out=a_out, in0=view(1), scalar1=1.0, scalar2=None, op0=ALU.add),
                  r=["mt"], w=["A1"])
            fw.op('dve', lambda e: e.tensor_tensor(out=a_out, in0=a_out, in1=nmv, op=ALU.mult), r=["A1", "nm"], w=["A1"])
            fw.op('dve', lambda e: e.tensor_copy(out=b_out, in_=view(0)), r=["mt"], w=["B1"])
        fw.barrier()


def stage1(fw, c, T, G):
    nc = fw.nc
    D, KC, TPC, CT, TW = c.D, c.KC, c.TPC, c.CT, c.TW
    RPB = 4
    ph, sb, ps = _phase(nc)
    with ph:
        xt = [sb("s1x%d" % i, [128, D]) for i in range(2)]
        xs = [sb("s1xs%d" % i, [128, D], BF16) for i in range(2)]
        jk = sb("s1jk", [128, D], BF16)
        ss = sb("s1ss", [128, 4]); rs = sb("s1rs", [128, 4])
        hx = [sb("s1hx%d" % i, [128, KC, 128], BF16) for i in range(2)]
        pT = [ps("s1pT%d" % i, [128, 1024], BF16) for i in range(2)]
        Ab = sb("s1Ab", [128, KC]); Bb = sb("s1Bb", [128, KC]); tmp = sb("s1tmp", [128, KC])
        for (dst, src) in ((Ab, G["A1"]), (Bb, G["B1"])):
            fw.op('dve', lambda e: e.tensor_tensor(out=tmp[:], in0=src[:, 1, :], in1=src[:, 0, :], op=ALU.subtract),
                  r=["A1", "B1", "rk"], w=["tmp"])
            fw.op('dve', lambda e: e.scalar_tensor_tensor(out=dst[:], in0=tmp[:], scalar=G["rk"][:, 1:2], in1=src[:, 0, :],
                                                          op0=ALU.mult, op1=ALU.add), r=["tmp", "A1", "B1", "rk"], w=["AB"])
        tiles = [(T["xtok"], t * 128, 128, t * 128, 0) for t in range(TPC // 128)] + [(T["ctok"], 0, CT, TPC, 1)]
        for ti, (src, r0, n, c0, isctx) in enumerate(tiles):
            b = ti % 2
            fw.dma('sp', xt[b][:n, :], src[r0:r0 + n, :], w=[("xt", b)])
            fw.op('act', lambda e: e.activation(out=jk[:n, :], in_=xt[b][:n, :], func=AF.Square, accum_out=ss[:n, b:b + 1]),
                  r=[("xt", b)], w=["jk", ("ss", b)])
            fw.op('dve', lambda e: e.tensor_scalar(out=rs[:n, b:b + 1], in0=ss[:n, b:b + 1], scalar1=1.0 / D, scalar2=EPS,
                                                   op0=ALU.mult, op1=ALU.add), r=[("ss", b)], w=[("rs", b)])
            fw.op('act', lambda e: e.activation(out=rs[:n, b:b + 1], in_=rs[:n, b:b + 1], func=AF.Sqrt), r=[("rs", b)], w=[("rs", b)])
            fw.op('dve', lambda e: e.reciprocal(out=rs[:n, b:b + 1], in_=rs[:n, b:b + 1]), r=[("rs", b)], w=[("rs", b)])
            fw.op('act', lambda e: e.activation(out=xs[b][:n, :], in_=xt[b][:n, :], func=AF.Copy, scale=rs[:n, b:b + 1]),
                  r=[("xt", b), ("rs", b)], w=[("xs", b)])
            for kg in range(KC // 8):
                pb = kg % 2
                for j in range(8):
                    kc = kg * 8 + j
                    fw.op('pe', lambda e: e.transpose(out=pT[pb][:, j * 128:j * 128 + n], in_=xs[b][:n, kc * 128:(kc + 1) * 128],
                                                      identity=G["ident_b"][:n, :n]), r=[("xs", b)], w=[("pT", pb)])
                for j in range(8):
                    kc = kg * 8 + j
                    if isctx:
                        sc_ap, bi_ap = G["A1"][:, 2, kc:kc + 1], G["B1"][:, 2, kc:kc + 1]
                    else:
                        sc_ap, bi_ap = Ab[:, kc:kc + 1], Bb[:, kc:kc + 1]
                    if j % 2 == 0:
                        fw.op('act', lambda e: e.activation(out=hx[b][:, kc, :n], in_=pT[pb][:, j * 128:j * 128 + n], func=AF.Identity,
                                                            scale=sc_ap, bias=bi_ap), r=[("pT", pb), "AB", "A1", "B1"], w=[("hx", b)])
                    else:
                        fw.op('dve', lambda e: e.tensor_scalar(out=hx[b][:, kc, :n], in0=pT[pb][:, j * 128:j * 128 + n], scalar1=sc_ap,
                                                               scalar2=bi_ap, op0=ALU.mult, op1=ALU.add),
                              r=[("pT", pb), "AB", "A1", "B1"], w=[("hx", b)])
            fw.dma('sp', T["hxT_loc"][:, c0:c0 + n].rearrange("(kc p) t -> p kc t", p=128), hx[b][:, :, :n],
                   r=[("hx", b)], w=["hxT_loc"])
        fw.allgather(T["hxT_loc"], T["hxT_all"], r=["hxT_loc"], w=["hxT_all"])
        fw.barrier()


def _tok_tiles(c):
    out = []
    nt = min(512, c.TPC)
    for r in range(8):
        b = r // 4
        for t in range(c.TPC // nt):
            s0 = (r % 4) * c.TPC + t * nt
            out.append(dict(r=r, c0=t * nt, n=nt, ctx=0, ncol=b * c.S + s0, kcol=b * c.L + s0, s0=s0))
        p0 = (r % 4) * c.CT
        out.append(dict(r=r, c0=c.TPC, n=c.CT, ctx=1, ncol=None, kcol=b * c.L + c.S + p0, s0=None))
    return out


def _load_w(fw, T, name, wsb, KC, c0, c1, key):
    for kc in range(KC):
        fw.dma('pool', wsb[:, kc, :c1 - c0], T[name][kc * 128:(kc + 1) * 128, c0:c1], w=[key])


def stage2(fw, c, T, G):
    nc = fw.nc
    D, KC, DS, CD, HPG, HQ, QF, NCC = c.D, c.KC, c.DS, c.CD, c.HPG, c.HQ, c.QF, c.NCC
    tiles = _tok_tiles(c)
    NTM = min(512, c.TPC)

    def load_hx(hx, ti, tl):
        b = ti % 2
        n = tl["n"]
        src = T["hxT_all"][tl["r"] * D:(tl["r"] + 1) * D, tl["c0"]:tl["c0"] + n].rearrange("(kc p) t -> p kc t", p=128)
        fw.dma('sp', hx[b][:, :, :n], src, r=["hxT_all"], w=[("hx", b)])
        return b, n

    import os
    ph, sb, ps = _phase(nc)
    with ph:
      if not os.environ.get("K_SKIPA"):
          NA = QF + 256
          wsb = sb("s2w", [128, KC, NA], BF16); wdt = sb("s2wdt", [128, KC, 2 * HPG], BF16)
          hx = [sb("s2hx%d" % i, [128, KC, NTM], BF16) for i in range(2)]
          rc = sb("s2rc", [128, NTM]); rs = sb("s2rs", [128, NTM]); qkw = sb("s2qkw", [128, 2])
          sqb = sb("s2sqb", [128, NTM], BF16); rstd = sb("s2rstd", [128, NTM]); qw = sb("s2qw", [128, NTM], BF16)
          t1 = sb("s2t1", [128, NTM]); t2 = sb("s2t2", [128, NTM]); qf = sb("s2qf", [128, NTM], BF16)
          vsb = sb("s2v", [128, 128], BF16); dsb = sb("s2dt", [128, 2 * HPG])
          pq = [ps("s2pq%d" % i, [128, 512]) for i in range(2)]
          pss = ps("s2pss", [128, 512]); psw = ps("s2psw", [128, 512]); pv = ps("s2pv", [128, 128]); pdt = ps("s2pdt", [128, 2 * HPG])
          _load_w(fw, T, "w1", wsb, KC, c.oq, c.oq + NA, "wsb")
          _load_w(fw, T, "w1", wdt, KC, c.odt, c.odt + 2 * HPG, "wdt")
          fw.dma('sp', qkw[:], T["qknw"][:, :], w=["qkw"])
          it = 0
          for ti, tl in enumerate(tiles):
              b, n = load_hx(hx, ti, tl)
              isctx = tl["ctx"]
              if not isctx:
                  fw.dma('sp', rc[:, :n], T["ropec"][:, tl["s0"]:tl["s0"] + n], w=["rc"])
                  fw.dma('sp', rs[:, :n], T["ropes"][:, tl["s0"]:tl["s0"] + n], w=["rs"])
              for hh in ([HQ] if isctx else list(range(HQ + 1))):
                  pb = it % 2; it += 1
                  wcol = 0 if hh < HQ else 1
                  for kc in range(KC):
                      fw.op('pe', lambda e: e.matmul(pq[pb][:, :n], lhsT=wsb[:, kc, hh * 128:(hh + 1) * 128], rhs=hx[b][:, kc, :n],
                                                     start=(kc == 0), stop=(kc == KC - 1)), r=["wsb", ("hx", b)], w=[("pq", pb)])
                  fw.op('act', lambda e: e.activation(out=sqb[:, :n], in_=pq[pb][:, :n], func=AF.Square), r=[("pq", pb)], w=["sqb"])
                  fw.op('pe', lambda e: e.matmul(pss[:, :n], lhsT=G["ones_b"], rhs=sqb[:, :n], start=True, stop=True),
                        r=["sqb", "cm_b"], w=["pss"])
                  fw.op('dve', lambda e: e.tensor_scalar(out=rstd[:, :n], in0=pss[:, :n], scalar1=1.0 / 128, scalar2=EPS,
                                                         op0=ALU.mult, op1=ALU.add), r=["pss"], w=["rstd"])
                  fw.op('act', lambda e: e.activation(out=rstd[:, :n], in_=rstd[:, :n], func=AF.Sqrt), r=["rstd"], w=["rstd"])
                  fw.op('dve', lambda e: e.reciprocal(out=rstd[:, :n], in_=rstd[:, :n]), r=["rstd"], w=["rstd"])
                  if isctx:
                      fw.op('dve', lambda e: e.scalar_tensor_tensor(out=qf[:, :n], in0=pq[pb][:, :n], scalar=qkw[:, 1:2], in1=rstd[:, :n],
                                                                    op0=ALU.mult, op1=ALU.mult), r=[("pq", pb), "qkw", "rstd"], w=["qf"])
                  else:
                      fw.op('act', lambda e: e.activation(out=qw[:, :n], in_=pq[pb][:, :n], func=AF.Copy, scale=qkw[:, wcol:wcol + 1]),
                            r=[("pq", pb), "qkw"], w=["qw"])
                      fw.op('pe', lambda e: e.matmul(psw[:, :n], lhsT=G["perm_b"], rhs=qw[:, :n], start=True, stop=True),
                            r=["qw", "cm_b"], w=["psw"])
                      fw.op('pool', lambda e: e.tensor_tensor(out=t1[:, :n], in0=qw[:, :n], in1=rc[:, :n], op=ALU.mult),
                            r=["qw", "rc"], w=["t1"])
                      fw.op('dve', lambda e: e.tensor_tensor(out=t2[:, :n], in0=psw[:, :n], in1=rs[:, :n], op=ALU.mult),
                            r=["psw", "rs"], w=["t2"])
                      fw.op('pool', lambda e: e.tensor_tensor(out=t1[:, :n], in0=t1[:, :n], in1=t2[:, :n], op=ALU.add),
                            r=["t1", "t2"], w=["t1"])
                      fw.op('dve', lambda e: e.tensor_tensor(out=qf[:, :n], in0=t1[:, :n], in1=rstd[:, :n], op=ALU.mult),
                            r=["t1", "rstd"], w=["qf"])
                  if hh < HQ:
                      fw.dma('sp', T["qT"][hh * 128:(hh + 1) * 128, tl["ncol"]:tl["ncol"] + n], qf[:, :n], r=["qf"], w=["qT"])
                  else:
                      fw.dma('sp', T["kT"][:, tl["kcol"]:tl["kcol"] + n], qf[:, :n], r=["qf"], w=["kT"])
              for sbk in range(_ceil(n, 128)):
                  m = min(128, n - sbk * 128)
                  for kc in range(KC):
                      fw.op('pe', lambda e: e.matmul(pv[:m, :], lhsT=hx[b][:, kc, sbk * 128:sbk * 128 + m], rhs=wsb[:, kc, QF + 128:QF + 256],
                                                     start=(kc == 0), stop=(kc == KC - 1)), r=["wsb", ("hx", b)], w=["pv"])
                  fw.op('act', lambda e: e.activation(out=vsb[:m, :], in_=pv[:m, :], func=AF.Copy), r=["pv"], w=["vsb"])
                  fw.dma('sp', T["vv"][tl["kcol"] + sbk * 128: tl["kcol"] + sbk * 128 + m, :], vsb[:m, :], r=["vsb"], w=["vv"])
                  for kc in range(KC):
                      fw.op('pe', lambda e: e.matmul(pdt[:m, :], lhsT=hx[b][:, kc, sbk * 128:sbk * 128 + m], rhs=wdt[:, kc, :],
                                                     start=(kc == 0), stop=(kc == KC - 1)), r=["wdt", ("hx", b)], w=["pdt"])
                  fw.op('dve', lambda e: e.tensor_copy(out=dsb[:m, :], in_=pdt[:m, :]), r=["pdt"], w=["dsb"])
                  fw.dma('sp', T["dtr"][tl["kcol"] + sbk * 128: tl["kcol"] + sbk * 128 + m, :], dsb[:m, :], r=["dsb"], w=["dtr"])
          fw.barrier()
    import os
    if os.environ.get("K_STOP", "99") == "2A":
        return

    ph, sb, ps = _phase(nc)
    with ph:
        NB = DS + 256
        wsb = sb("s2bw", [128, KC, NB], BF16)
        hx = [sb("s2bhx%d" % i, [128, KC, NTM], BF16) for i in range(2)]
        xb = [sb("s2bx%d" % i, [128, NTM]) for i in range(2)]
        pq = [ps("s2bp%d" % i, [128, 512]) for i in range(2)]
        _load_w(fw, T, "w1", wsb, KC, c.ox, c.ox + NB, "wsb")
        it = 0
        for ti, tl in enumerate(tiles):
            b, n = load_hx(hx, ti, tl)
            for cb in range(NCC):
                pb = it % 2; it += 1
                for kc in range(KC):
                    fw.op('pe', lambda e: e.matmul(pq[pb][:, :n], lhsT=wsb[:, kc, cb * 128:(cb + 1) * 128], rhs=hx[b][:, kc, :n],
                                                   start=(kc == 0), stop=(kc == KC - 1)), r=["wsb", ("hx", b)], w=[("pq", pb)])
                if pb == 0:
                    fw.op('act', lambda e: e.activation(out=xb[pb][:, :n], in_=pq[pb][:, :n], func=AF.Copy), r=[("pq", pb)], w=[("xb", pb)])
                else:
                    fw.op('dve', lambda e: e.tensor_copy(out=xb[pb][:, :n], in_=pq[pb][:, :n]), r=[("pq", pb)], w=[("xb", pb)])
                fw.dma('sp', T["xbc"][cb * 128:(cb + 1) * 128, tl["kcol"]:tl["kcol"] + n], xb[pb][:, :n], r=[("xb", pb)], w=["xbc"])
        fw.barrier()
    if os.environ.get("K_STOP", "99") == "2B":
        return

    ph, sb, ps = _phase(nc)
    with ph:
        wsb = sb("s2cw", [128, KC, 3 * DS], BF16)
        hx = [sb("s2chx%d" % i, [128, KC, NTM], BF16) for i in range(2)]
        gb = [sb("s2cg%d" % i, [128, NTM], BF16) for i in range(2)]
        zb = [sb("s2cz%d" % i, [128, DS]) for i in range(2)]
        pq = [ps("s2cp%d" % i, [128, 512]) for i in range(2)]
        pz = [ps("s2cpz%d" % i, [128, 512]) for i in range(2)]
        _load_w(fw, T, "w1", wsb, KC, c.oga, c.oga + 3 * DS, "wsb")
        it = 0
        li = 0
        for tl in tiles:
            if tl["ctx"]:
                continue
            b, n = load_hx(hx, li, tl); li += 1
            for cb in range(2 * CD):
                pb = it % 2; it += 1
                for kc in range(KC):
                    fw.op('pe', lambda e: e.matmul(pq[pb][:, :n], lhsT=wsb[:, kc, cb * 128:(cb + 1) * 128], rhs=hx[b][:, kc, :n],
                                                   start=(kc == 0), stop=(kc == KC - 1)), r=["wsb", ("hx", b)], w=[("pq", pb)])
                fw.op('act', lambda e: e.activation(out=gb[pb][:, :n], in_=pq[pb][:, :n], func=AF.Sigmoid), r=[("pq", pb)], w=[("gb", pb)])
                dst = T["gaT"] if cb < CD else T["gsT"]
                rr = (cb % CD) * 128
                fw.dma('sp', dst[rr:rr + 128, tl["ncol"]:tl["ncol"] + n], gb[pb][:, :n], r=[("gb", pb)], w=["gT"])
            for sbk in range(n // 128):
                pb = it % 2; it += 1
                for kc in range(KC):
                    fw.op('pe', lambda e: e.matmul(pz[pb][:, :DS], lhsT=hx[b][:, kc, sbk * 128:(sbk + 1) * 128], rhs=wsb[:, kc, 2 * DS:3 * DS],
                                                   start=(kc == 0), stop=(kc == KC - 1)), r=["wsb", ("hx", b)], w=[("pz", pb)])
                fw.op('act', lambda e: e.activation(out=zb[pb][:, :], in_=pz[pb][:, :DS], func=AF.Silu), r=[("pz", pb)], w=[("zb", pb)])
                fw.dma('sp', T["szt"][tl["ncol"] + sbk * 128: tl["ncol"] + (sbk + 1) * 128, :], zb[pb][:, :], r=[("zb", pb)], w=["szt"])
        fw.barrier()


def stage3(fw, c, T, G):
    nc = fw.nc
    S, L, HQ = c.S, c.L, c.HQ
    LT = L // 128
    QB = min(512, S)
    scale = 128 ** -0.5
    ph, sb, ps = _phase(nc)
    with ph:
        kt = sb("s3k", [128, L], BF16); vt = sb("s3v", [128, LT, 128], BF16)
        qt = [sb("s3q%d" % i, [128, QB], BF16) for i in range(2)]
        pt = [sb("s3p%d" % i, [128, QB], BF16) for i in range(3)]
        acc = [sb("s3acc%d" % i, [128, QB]) for i in range(2)]
        rcp = sb("s3rcp", [128, QB]); ob = [sb("s3o%d" % i, [128, QB], BF16) for i in range(2)]
        pS = [ps("s3ps%d" % i, [128, 512]) for i in range(2)]
        pO = [ps("s3po%d" % i, [128, 512]) for i in range(2)]
        pD = [ps("s3pd%d" % i, [128, 512]) for i in range(2)]
        it = 0
        qi = 0
        for b in range(2):
            fw.dma('sp', kt[:, :], T["kT"][:, b * L:(b + 1) * L], w=["kt"])
            fw.dma('sp', vt[:, :, :], T["vv"][b * L:(b + 1) * L, :].rearrange("(t p) d -> p t d", p=128), w=["vt"])
            for h in range(HQ):
                for qb in range(S // QB):
                    qq = qi % 2; qi += 1
                    col = b * S + qb * QB
                    fw.dma('sp', qt[qq][:, :], T["qT"][h * 128:(h + 1) * 128, col:col + QB], r=["qT"], w=[("qt", qq)])
                    def smm(t):
                        fw.op('pe', lambda e: e.matmul(pS[t % 2][:, :QB], lhsT=kt[:, t * 128:(t + 1) * 128], rhs=qt[qq][:, :],
                                                       start=True, stop=True), r=["kt", ("qt", qq)], w=[("pS", t % 2)])
                    smm(0)
                    for t in range(LT):
                        pi = it % 3; it += 1
                        if t + 1 < LT:
                            smm(t + 1)
                        fw.op('act', lambda e: e.activation(out=pt[pi][:, :], in_=pS[t % 2][:, :QB], func=AF.Exp, scale=scale),
                              r=[("pS", t % 2)], w=[("pt", pi)])
                        fw.op('pe', lambda e: e.matmul(pO[qq][:, :QB], lhsT=vt[:, t, :], rhs=pt[pi][:, :], start=(t == 0), stop=(t == LT - 1)),
                              r=["vt", ("pt", pi)], w=[("pO", qq)])
                        if t == 0:
                            fw.op('dve', lambda e: e.tensor_copy(out=acc[qq][:, :], in_=pt[pi][:, :]), r=[("pt", pi)], w=[("acc", qq)])
                        else:
                            fw.op('dve', lambda e: e.tensor_tensor(out=acc[qq][:, :], in0=acc[qq][:, :], in1=pt[pi][:, :], op=ALU.add),
                                  r=[("pt", pi), ("acc", qq)], w=[("acc", qq)])
                    fw.op('pe', lambda e: e.matmul(pD[qq][:, :QB], lhsT=G["ones_f"], rhs=acc[qq][:, :], start=True, stop=True),
                          r=["cm_f", ("acc", qq)], w=[("pD", qq)])
                    fw.op('dve', lambda e: e.reciprocal(out=rcp[:, :], in_=pD[qq][:, :QB]), r=[("pD", qq)], w=["rcp"])
                    fw.op('dve', lambda e: e.tensor_tensor(out=ob[qq][:, :], in0=pO[qq][:, :QB], in1=rcp[:, :], op=ALU.mult),
                          r=[("pO", qq), "rcp"], w=[("ob", qq)])
                    fw.dma('sp', T["ATloc"][h * 128:(h + 1) * 128, col:col + QB], ob[qq][:, :], r=[("ob", qq)], w=["ATloc"])
        fw.allgather(T["ATloc"], T["ATall"], r=["ATloc"], w=["ATall"])
        fw.barrier()


def stage4(fw, c, T, G):
    nc = fw.nc
    S, CTX, L, DS, CD, HPG, NCC, NTT, QF, D = c.S, c.CTX, c.L, c.DS, c.CD, c.HPG, c.NCC, c.NTT, c.QF, c.D
    H2 = 2 * HPG
    NCH = S // 128
    SEG = min(2048, S)
    ph, sb, ps = _phase(nc)
    with ph:
        cw = sb("s4cw", [128, NCC * 6]); svb = sb("s4sv", [128, 5 * HPG]); aval = sb("s4a", [128, H2])
        raw = sb("s4raw", [128, SEG + 4]); acc = sb("s4acc", [128, SEG]); xT = sb("s4xT", [128, S], BF16)
        xbf = sb("s4xbf", [128, NCH, DS], BF16); BT = sb("s4BT", [128, S], BF16); CTt = sb("s4CT", [128, S], BF16)
        Btm = sb("s4Btm", [128, NCH, 128], BF16)
        cbm = [sb("s4cbm%d" % i, [128, NCH, 128], BF16) for i in range(2)]
        dtt = sb("s4dt", [128, NCH, H2]); da = sb("s4da", [128, NCH, H2])
        hT = [sb("s4h%d" % i, [128, DS]) for i in range(2)]; hTb = [sb("s4hb%d" % i, [128, DS], BF16) for i in range(2)]
        acs = sb("s4acs", [128, H2]); te = sb("s4te", [128, HPG]); el = sb("s4el", [128, HPG])
        xsc = sb("s4xsc", [128, DS], BF16)
        sg = [sb("s4sg%d" % i, [128, 128]) for i in range(2)]; wt = [sb("s4wt%d" % i, [128, 128], BF16) for i in range(2)]
        ebc = [sb("s4ebc%d" % i, [128, 128]) for i in range(2)]; ce = [sb("s4ce%d" % i, [128, 128], BF16) for i in range(2)]
        ysb = sb("s4y", [128, DS]); yl = sb("s4yl", [128, DS]); zl = sb("s4zl", [128, DS]); jk = sb("s4jk", [128, DS], BF16)
        ss1 = sb("s4ss", [128, NTT])
        pT = ps("s4pT", [128, 1024], BF16); pCB = ps("s4pCB", [128, 128]); pA = ps("s4pA", [128, H2])
        pB = [ps("s4pB%d" % i, [128, 128]) for i in range(2)]; pY = ps("s4pY", [128, 512]); pU = ps("s4pU", [128, 512])
        fw.dma('sp', cw[:], T["convw"][:, :], w=["cw"])
        fw.dma('sp', svb[:], T["ssdv"][0:1, :].partition_broadcast(128), w=["svb"])
        fw.op('act', lambda e: e.activation(out=aval[:], in_=svb[:, 0:H2], func=AF.Exp), r=["svb"], w=["aval"])
        fw.op('dve', lambda e: e.tensor_scalar(out=aval[:], in0=aval[:], scalar1=-1.0, scalar2=None, op0=ALU.mult), r=["aval"], w=["aval"])
        dsk = svb[:, 4 * HPG:5 * HPG]
        hcnt = [0]

        def prepass(b, isctx):
            Ls = CTX if isctx else S
            nch = Ls // 128
            col0 = b * L + (S if isctx else 0)
            for cc in range(NCC):
                if isctx and cc == NCC - 1:
                    continue
                for s0 in range(0, Ls, SEG):
                    n = min(SEG, Ls - s0)
                    lo = max(0, s0 - 2); hi = min(Ls, s0 + n + 2)
                    fw.op('pool', lambda e: e.memset(raw[:, :], 0.0), w=["raw"])
                    fw.dma('sp', raw[:, 2 - (s0 - lo): 2 - (s0 - lo) + (hi - lo)], T["xbc"][cc * 128:(cc + 1) * 128, col0 + lo: col0 + hi],
                           r=["xbc"], w=["raw"])
                    fw.op('dve', lambda e: e.tensor_scalar(out=acc[:, :n], in0=raw[:, 0:n], scalar1=cw[:, cc * 6:cc * 6 + 1], scalar2=None,
                                                           op0=ALU.mult), r=["raw", "cw"], w=["acc"])
                    for j in range(1, 5):
                        fw.op('dve', lambda e: e.scalar_tensor_tensor(out=acc[:, :n], in0=raw[:, j:j + n], scalar=cw[:, cc * 6 + j:cc * 6 + j + 1],
                                                                      in1=acc[:, :n], op0=ALU.mult, op1=ALU.add), r=["raw", "cw", "acc"], w=["acc"])
                    dst = xT if cc < CD else (BT if cc == CD else CTt)
                    fw.op('act', lambda e: e.activation(out=dst[:, s0:s0 + n], in_=acc[:, :n], func=AF.Silu, bias=cw[:, cc * 6 + 5:cc * 6 + 6]),
                          r=["acc", "cw"], w=["cv%d" % min(cc, CD + 1) if cc >= CD else "xT"])
                if cc <= CD:
                    src = xT if cc < CD else BT
                    skey = "xT" if cc < CD else "cv%d" % CD
                    for t0 in range(0, nch, 8):
                        m = min(8, nch - t0)
                        for t in range(t0, t0 + m):
                            fw.op('pe', lambda e: e.transpose(out=pT[:, (t - t0) * 128:(t - t0 + 1) * 128], in_=src[:, t * 128:(t + 1) * 128],
                                                              identity=G["ident_b"]), r=[skey, "cm_b"], w=["pT"])
                        pv = pT[:, :m * 128].rearrange("p (t k) -> p t k", k=128)
                        if cc < CD:
                            fw.op('act', lambda e: e.activation(out=xbf[:, t0:t0 + m, cc * 128:(cc + 1) * 128], in_=pv, func=AF.Copy),
                                  r=["pT"], w=["xbf"])
                        else:
                            fw.op('act', lambda e: e.activation(out=Btm[:, t0:t0 + m, :], in_=pv, func=AF.Copy), r=["pT"], w=["Btm"])
            fw.dma('sp', dtt[:, :nch, :], T["dtr"][col0:col0 + Ls, :].rearrange("(t p) h -> p t h", p=128), r=["dtr"], w=["dtt"])
            fw.op('dve', lambda e: e.tensor_tensor(out=dtt[:, :nch, :], in0=dtt[:, :nch, :],
                                                   in1=svb[:, 2 * HPG:4 * HPG].unsqueeze(1).to_broadcast([128, nch, H2]), op=ALU.add),
                  r=["dtt", "svb"], w=["dtt"])
            fw.op('act', lambda e: e.activation(out=dtt[:, :nch, :], in_=dtt[:, :nch, :], func=AF.Exp), r=["dtt"], w=["dtt"])
            fw.op('dve', lambda e: e.tensor_scalar(out=dtt[:, :nch, :], in0=dtt[:, :nch, :], scalar1=1.0, scalar2=None, op0=ALU.add),
                  r=["dtt"], w=["dtt"])
            fw.op('act', lambda e: e.activation(out=dtt[:, :nch, :], in_=dtt[:, :nch, :], func=AF.Ln), r=["dtt"], w=["dtt"])
            fw.op('dve', lambda e: e.tensor_tensor(out=da[:, :nch, :], in0=dtt[:, :nch, :],
                                                   in1=aval[:, :].unsqueeze(1).to_broadcast([128, nch, H2]), op=ALU.mult),
                  r=["dtt", "aval"], w=["da"])
            if not isctx:
                for t in range(nch):
                    fw.op('pe', lambda e: e.matmul(pCB[:, :], lhsT=BT[:, t * 128:(t + 1) * 128], rhs=CTt[:, t * 128:(t + 1) * 128],
                                                   start=True, stop=True), r=["cv%d" % CD, "cv%d" % (CD + 1)], w=["pCB"])
                    fw.op('dve', lambda e: e.tensor_tensor(out=cbm[0][:, t, :], in0=pCB[:, :], in1=G["triinc_f"], op=ALU.mult),
                          r=["pCB", "cm_f"], w=["cbm0"])
                    fw.op('dve', lambda e: e.tensor_tensor(out=cbm[1][:, t, :], in0=pCB[:, :], in1=G["tridec_f"], op=ALU.mult),
                          r=["pCB", "cm_f"], w=["cbm1"])

        def chunk(b, isctx, d, t):
            tri = G["triinc_f"] if d == 0 else G["tridec_f"]
            dsl = slice(d * HPG, (d + 1) * HPG)
            hk = "hT%d" % d
            fw.op('pe', lambda e: e.matmul(pA[:, 0:HPG], lhsT=tri, rhs=da[:, t, dsl], start=True, stop=True), r=["da", "cm_f"], w=["pA"])
            fw.op('pe', lambda e: e.matmul(pA[:, HPG:H2], lhsT=G["ones_f"], rhs=da[:, t, dsl], start=True, stop=True), r=["da", "cm_f"], w=["pA"])
            fw.op('dve', lambda e: e.tensor_copy(out=acs[:, :], in_=pA[:, :]), r=["pA"], w=["acs"])
            fw.op('dve', lambda e: e.tensor_tensor(out=te[:, :], in0=acs[:, HPG:H2], in1=acs[:, 0:HPG], op=ALU.subtract), r=["acs"], w=["te"])
            fw.op('act', lambda e: e.activation(out=te[:, :], in_=te[:, :], func=AF.Exp), r=["te"], w=["te"])
            fw.op('dve', lambda e: e.tensor_tensor(out=te[:, :], in0=te[:, :], in1=dtt[:, t, dsl], op=ALU.mult), r=["te", "dtt"], w=["te"])
            fw.op('act', lambda e: e.activation(out=el[:, :], in_=acs[:, HPG:H2], func=AF.Exp), r=["acs"], w=["el"])
            fw.op('pool', lambda e: e.tensor_tensor(out=xsc[:, :].rearrange("p (h k) -> p h k", k=64),
                                                    in0=xbf[:, t, :].rearrange("p (h k) -> p h k", k=64),
                                                    in1=te[:, :].unsqueeze(2).to_broadcast([128, HPG, 64]), op=ALU.mult),
                  r=["xbf", "te"], w=["xsc"])
            if not isctx:
                for h in range(HPG):
                    i2 = hcnt[0] % 2; hcnt[0] += 1
                    hs = slice(h * 64, (h + 1) * 64)
                    fw.op('pe', lambda e: e.matmul(pB[i2][:, :], lhsT=da[:, t, d * HPG + h:d * HPG + h + 1].to_broadcast([128, 128]), rhs=tri,
                                                   start=True, stop=True), r=["da", "cm_f"], w=[("pB", i2)])
                    fw.op('dve', lambda e: e.tensor_scalar(out=sg[i2][:, :], in0=pB[i2][:, :], scalar1=acs[:, h:h + 1], scalar2=0.0,
                                                           op0=ALU.subtract, op1=ALU.min), r=[("pB", i2), "acs"], w=[("sg", i2)])
                    fw.op('act', lambda e: e.activation(out=sg[i2][:, :], in_=sg[i2][:, :], func=AF.Exp), r=[("sg", i2)], w=[("sg", i2)])
                    fw.op('dve', lambda e: e.scalar_tensor_tensor(out=wt[i2][:, :], in0=sg[i2][:, :], scalar=dtt[:, t, d * HPG + h:d * HPG + h + 1],
                                                                  in1=cbm[d][:, t, :], op0=ALU.mult, op1=ALU.mult),
                          r=[("sg", i2), "dtt", "cbm%d" % d], w=[("wt", i2)])
                    fw.op('pe', lambda e: e.matmul(pY[:, hs], lhsT=wt[i2][:, :], rhs=xbf[:, t, hs], start=True, stop=False),
                          r=[("wt", i2), "xbf"], w=["pY"])
                    fw.op('act', lambda e: e.activation(out=ebc[i2][:, :], in_=pB[i2][:, :], func=AF.Exp), r=[("pB", i2)], w=[("ebc", i2)])
                    fw.op('pool', lambda e: e.tensor_tensor(out=ce[i2][:, :], in0=CTt[:, t * 128:(t + 1) * 128], in1=ebc[i2][:, :], op=ALU.mult),
                          r=["cv%d" % (CD + 1), ("ebc", i2)], w=[("ce", i2)])
                    fw.op('pe', lambda e: e.matmul(pY[:, hs], lhsT=ce[i2][:, :], rhs=hTb[d][:, hs], start=False, stop=True),
                          r=[("ce", i2), hk + "b"], w=["pY"])
                tt = (b * S) // 128 + t
                rows = slice(b * S + t * 128, b * S + (t + 1) * 128)
                if d == 0:
                    fw.op('dve', lambda e: e.tensor_tensor(out=ysb[:, :].rearrange("p (h k) -> p h k", k=64),
                                                           in0=xbf[:, t, :].rearrange("p (h k) -> p h k", k=64),
                                                           in1=dsk.unsqueeze(2).to_broadcast([128, HPG, 64]), op=ALU.mult),
                          r=["xbf", "svb"], w=["ysb"])
                    fw.op('dve', lambda e: e.tensor_tensor(out=ysb[:, :], in0=ysb[:, :], in1=pY[:, :DS], op=ALU.add), r=["ysb", "pY"], w=["ysb"])
                    fw.dma('sp', T["ygs"][rows, :], ysb[:, :], r=["ysb"], w=[("ygs", tt)])
                else:
                    fw.dma('sp', yl[:, :], T["ygs"][rows, :], r=[("ygs", tt)], w=["yl"])
                    fw.dma('sp', zl[:, :], T["szt"][rows, :], r=["szt"], w=["zl"])
                    fw.op('dve', lambda e: e.tensor_tensor(out=yl[:, :], in0=yl[:, :], in1=pY[:, :DS], op=ALU.add), r=["yl", "pY"], w=["yl"])
                    fw.op('pool', lambda e: e.tensor_tensor(out=yl[:, :], in0=yl[:, :], in1=zl[:, :], op=ALU.mult), r=["yl", "zl"], w=["yl"])
                    fw.op('act', lambda e: e.activation(out=jk[:, :], in_=yl[:, :], func=AF.Square, accum_out=ss1[:, tt:tt + 1]),
                          r=["yl"], w=["jk", "ss1"])
                    fw.dma('sp', T["ygs"][rows, :], yl[:, :], r=["yl"], w=[("ygs", tt)])
            fw.op('pe', lambda e: e.matmul(pU[:, :DS], lhsT=Btm[:, t, :], rhs=xsc[:, :], start=True, stop=True), r=["Btm", "xsc"], w=["pU"])
            for h in range(HPG):
                hs = slice(h * 64, (h + 1) * 64)
                fw.op('dve', lambda e: e.scalar_tensor_tensor(out=hT[d][:, hs], in0=hT[d][:, hs], scalar=el[:, h:h + 1], in1=pU[:, hs],
                                                              op0=ALU.mult, op1=ALU.add), r=[hk, "el", "pU"], w=[hk])
            fw.op('act', lambda e: e.activation(out=hTb[d][:, :], in_=hT[d][:, :], func=AF.Copy), r=[hk], w=[hk + "b"])

        for b in range(2):
            for d in range(2):
                fw.op('pool', lambda e: e.memset(hT[d][:, :], 0.0), w=["hT%d" % d])
                fw.op('pool', lambda e: e.memset(hTb[d][:, :], 0.0), w=["hT%db" % d])
            for isctx in (1, 0):
                prepass(b, isctx)
                nch = (CTX if isctx else S) // 128
                for d in range(2):
                    for t in (range(nch) if d == 0 else range(nch - 1, -1, -1)):
                        chunk(b, isctx, d, t)
        fw.dma('sp', T["ss1loc"][:, :], ss1[:, :], r=["ss1"], w=["ss1loc"])
        fw.allgather(T["ss1loc"], T["ss1all"], r=["ss1loc"], w=["ss1all"])
        fw.barrier()


def stage5(fw, c, T, G):
    nc = fw.nc
    DS, CD, NTT, QF, D = c.DS, c.CD, c.NTT, c.QF, c.D
    ph, sb, ps = _phase(nc)
    with ph:
        sa = sb("s5sa", [128, 8, NTT]); rst = sb("s5rst", [128, NTT]); snb = sb("s5snb", [128, DS])
        yg = [sb("s5yg%d" % i, [128, DS]) for i in range(2)]; yn = [sb("s5yn%d" % i, [128, DS], BF16) for i in range(2)]
        yT = [sb("s5yT%d" % i, [128, CD, 512], BF16) for i in range(2)]
        pT = [ps("s5pT%d" % i, [128, 1024], BF16) for i in range(2)]
        fw.dma('sp', sa[:, :, :], T["ss1all"][:, :].rearrange("(r p) t -> p r t", p=128), r=["ss1all"], w=["sa"])
        fw.dma('sp', snb[:, :], T["snw"][0:1, :].partition_broadcast(128), w=["snb"])
        fw.op('dve', lambda e: e.tensor_reduce(out=rst[:, :], in_=sa[:, :, :].rearrange("p r t -> p t r"), axis=AX.X, op=ALU.add),
              r=["sa"], w=["rst"])
        fw.op('dve', lambda e: e.tensor_scalar(out=rst[:, :], in0=rst[:, :], scalar1=1.0 / D, scalar2=EPS, op0=ALU.mult, op1=ALU.add),
              r=["rst"], w=["rst"])
        fw.op('act', lambda e: e.activation(out=rst[:, :], in_=rst[:, :], func=AF.Sqrt), r=["rst"], w=["rst"])
        fw.op('dve', lambda e: e.reciprocal(out=rst[:, :], in_=rst[:, :]), r=["rst"], w=["rst"])
        GT = min(4, NTT)
        for g0 in range(0, NTT, GT):
            gi = (g0 // GT) % 2
            for j in range(GT):
                tt = g0 + j
                b = tt % 2
                fw.dma('sp', yg[b][:, :], T["ygs"][tt * 128:(tt + 1) * 128, :], r=["ygs"], w=[("yg", b)])
                fw.op('dve', lambda e: e.scalar_tensor_tensor(out=yn[b][:, :], in0=yg[b][:, :], scalar=rst[:, tt:tt + 1], in1=snb[:, :],
                                                              op0=ALU.mult, op1=ALU.mult), r=[("yg", b), "rst", "snb"], w=[("yn", b)])
                for cc in range(CD):
                    fw.op('pe', lambda e: e.transpose(out=pT[b][:, cc * 128:(cc + 1) * 128], in_=yn[b][:, cc * 128:(cc + 1) * 128],
                                                      identity=G["ident_b"]), r=[("yn", b), "cm_b"], w=[("pT", b)])
                fw.op('act', lambda e: e.activation(out=yT[gi][:, :, j * 128:(j + 1) * 128],
                                                    in_=pT[b][:, :CD * 128].rearrange("p (c k) -> p c k", k=128), func=AF.Copy),
                      r=[("pT", b)], w=[("yT", gi)])
            for cc in range(CD):
                fw.dma('sp', T["YNloc"][cc * 128:(cc + 1) * 128, g0 * 128:(g0 + GT) * 128], yT[gi][:, cc, :GT * 128],
                       r=[("yT", gi)], w=["YNloc"])
        fw.allgather(T["YNloc"], T["YNall"], r=["YNloc"], w=["YNall"])
        fw.barrier()


def stage6(fw, c, T, G):
    nc = fw.nc
    DS, CD, KC, QF, QC, NT, D = c.DS, c.CD, c.KC, c.QF, c.QC, c.NT, c.D
    KA = 8 * QC
    N6 = 256
    ph, sb, ps = _phase(nc)
    with ph:
        wa = sb("s6wa", [128, KA, DS], BF16); ws = sb("s6ws", [128, KC, DS], BF16)
        at = [sb("s6at%d" % i, [128, KA, N6], BF16) for i in range(2)]
        yt = [sb("s6yt%d" % i, [128, KC, N6], BF16) for i in range(2)]
        ga = [sb("s6ga%d" % i, [128, N6], BF16) for i in range(2)]; gs = [sb("s6gs%d" % i, [128, N6], BF16) for i in range(2)]
        m1 = [sb("s6m1%d" % i, [128, N6]) for i in range(2)]; m2 = [sb("s6m2%d" % i, [128, N6]) for i in range(2)]
        mo = [sb("s6mo%d" % i, [128, N6], BF16) for i in range(2)]
        pA = [ps("s6pA%d" % i, [128, 512]) for i in range(2)]; pS = [ps("s6pS%d" % i, [128, 512]) for i in range(2)]
        _load_w(fw, T, "wap", wa, KA, 0, DS, "wa")
        _load_w(fw, T, "wsp", ws, KC, 0, DS, "ws")
        it = 0
        for ti, n0 in enumerate(range(0, NT, N6)):
            b = ti % 2
            for r in range(8):
                fw.dma('sp', at[b][:, r * QC:(r + 1) * QC, :], T["ATall"][r * QF:(r + 1) * QF, n0:n0 + N6].rearrange("(q p) n -> p q n", p=128),
                       r=["ATall"], w=[("at", b)])
                fw.dma('sp', yt[b][:, r * CD:(r + 1) * CD, :], T["YNall"][r * DS:(r + 1) * DS, n0:n0 + N6].rearrange("(q p) n -> p q n", p=128),
                       r=["YNall"], w=[("yt", b)])
            for cb in range(CD):
                i2 = it % 2; it += 1
                fw.dma('sp', ga[i2][:, :], T["gaT"][cb * 128:(cb + 1) * 128, n0:n0 + N6], r=["gT"], w=[("ga", i2)])
                fw.dma('sp', gs[i2][:, :], T["gsT"][cb * 128:(cb + 1) * 128, n0:n0 + N6], r=["gT"], w=[("gs", i2)])
                for kc in range(KA):
                    fw.op('pe', lambda e: e.matmul(pA[i2][:, :N6], lhsT=wa[:, kc, cb * 128:(cb + 1) * 128], rhs=at[b][:, kc, :],
                                                   start=(kc == 0), stop=(kc == KA - 1)), r=["wa", ("at", b)], w=[("pA", i2)])
                for kc in range(KC):
                    fw.op('pe', lambda e: e.matmul(pS[i2][:, :N6], lhsT=ws[:, kc, cb * 128:(cb + 1) * 128], rhs=yt[b][:, kc, :],
                                                   start=(kc == 0), stop=(kc == KC - 1)), r=["ws", ("yt", b)], w=[("pS", i2)])
                fw.op('dve', lambda e: e.tensor_tensor(out=m1[i2][:, :], in0=pA[i2][:, :N6], in1=ga[i2][:, :], op=ALU.mult),
                      r=[("pA", i2), ("ga", i2)], w=[("m1", i2)])
                fw.op('dve', lambda e: e.tensor_tensor(out=m2[i2][:, :], in0=pS[i2][:, :N6], in1=gs[i2][:, :], op=ALU.mult),
                      r=[("pS", i2), ("gs", i2)], w=[("m2", i2)])
                fw.op('pool', lambda e: e.tensor_tensor(out=mo[i2][:, :], in0=m1[i2][:, :], in1=m2[i2][:, :], op=ALU.add),
                      r=[("m1", i2), ("m2", i2)], w=[("mo", i2)])
                fw.dma('sp', T["mT_loc"][cb * 128:(cb + 1) * 128, n0:n0 + N6], mo[i2][:, :], r=[("mo", i2)], w=["mT_loc"])
        fw.allgather(T["mT_loc"], T["mT_all"], r=["mT_loc"], w=["mT_all"])
        fw.barrier()


def stage7(fw, c, T, G):
    nc = fw.nc
    DS, CD, KC, NT, NTT, D, S = c.DS, c.CD, c.KC, c.NT, c.NTT, c.D, c.S
    N7 = min(512, NT)
    ph, sb, ps = _phase(nc)
    with ph:
        wo = sb("s7wo", [128, KC, DS], BF16)
        mt = [sb("s7mt%d" % i, [128, KC, N7], BF16) for i in range(2)]
        gtm = [sb("s7gtm%d" % i, [128, DS]) for i in range(2)]
        xc = [sb("s7xc%d" % i, [128, DS]) for i in range(2)]; x1 = [sb("s7x1%d" % i, [128, DS]) for i in range(2)]
        jk = sb("s7jk", [128, DS], BF16); ss2 = sb("s7ss", [128, NTT])
        pO = [ps("s7pO%d" % i, [128, 512]) for i in range(2)]
        _load_w(fw, T, "wout", wo, KC, 0, DS, "wo")
        for b in range(2):
            fw.dma('sp', gtm[b][:, :], T["modloc"][b:b + 1, 2 * DS:3 * DS].partition_broadcast(128), r=["modloc"], w=["gtm"])
        it = 0
        for ti, n0 in enumerate(range(0, NT, N7)):
            mb = ti % 2
            fw.dma('sp', mt[mb][:, :, :], T["mT_all"][:, n0:n0 + N7].rearrange("(kc p) n -> p kc n", p=128), r=["mT_all"], w=[("mt", mb)])
            for sbk in range(N7 // 128):
                i2 = it % 2; it += 1
                r0 = n0 + sbk * 128
                tt = r0 // 128
                b = r0 // S
                fw.dma('sp', xc[i2][:, :], T["xcol"][r0:r0 + 128, :], w=[("xc", i2)])
                for kc in range(KC):
                    fw.op('pe', lambda e: e.matmul(pO[i2][:, :DS], lhsT=mt[mb][:, kc, sbk * 128:(sbk + 1) * 128], rhs=wo[:, kc, :],
                                                   start=(kc == 0), stop=(kc == KC - 1)), r=["wo", ("mt", mb)], w=[("pO", i2)])
                fw.op('dve', lambda e: e.tensor_tensor(out=x1[i2][:, :], in0=pO[i2][:, :DS], in1=gtm[b][:, :], op=ALU.mult),
                      r=[("pO", i2), "gtm"], w=[("x1", i2)])
                fw.op('pool', lambda e: e.tensor_tensor(out=x1[i2][:, :], in0=x1[i2][:, :], in1=xc[i2][:, :], op=ALU.add),
                      r=[("x1", i2), ("xc", i2)], w=[("x1", i2)])
                fw.op('act', lambda e: e.activation(out=jk[:, :], in_=x1[i2][:, :], func=AF.Square, accum_out=ss2[:, tt:tt + 1]),
                      r=[("x1", i2)], w=["jk", "ss2"])
                fw.dma('sp', T["x1"][r0:r0 + 128, :], x1[i2][:, :], r=[("x1", i2)], w=["x1d"])
        fw.dma('sp', T["ss2loc"][:, :], ss2[:, :], r=["ss2"], w=["ss2loc"])
        fw.allgather(T["ss2loc"], T["ss2all"], r=["ss2loc"], w=["ss2all"])
        fw.barrier()
    ph, sb, ps = _phase(nc)
    with ph:
        sa = sb("s7sa", [128, 8, NTT]); rst = sb("s7rst", [128, NTT]); nfb = sb("s7nfb", [128, DS])
        A2 = [sb("s7A2%d" % i, [128, DS]) for i in range(2)]; B2 = [sb("s7B2%d" % i, [128, DS]) for i in range(2)]
        x1 = [sb("s7bx%d" % i, [128, DS]) for i in range(2)]; hf = [sb("s7hf%d" % i, [128, DS]) for i in range(2)]
        hb = [sb("s7hb%d" % i, [128, DS], BF16) for i in range(2)]; hTf = [sb("s7hT%d" % i, [128, CD, 128]) for i in range(2)]
        wrf = sb("s7wr", [128, CD, 36]); lg = [sb("s7lg%d" % i, [128, 36]) for i in range(2)]
        pT = [ps("s7pT%d" % i, [128, 512]) for i in range(2)]; pL = [ps("s7pL%d" % i, [128, 36]) for i in range(2)]
        fw.dma('sp', sa[:, :, :], T["ss2all"][:, :].rearrange("(r p) t -> p r t", p=128), r=["ss2all"], w=["sa"])
        fw.dma('sp', nfb[:, :], T["nfw"][0:1, :].partition_broadcast(128), w=["nfb"])
        fw.dma('sp', wrf[:, :, :], T["wr"][:, :].rearrange("(c p) e -> p c e", p=128), w=["wrf"])
        for b in range(2):
            fw.dma('sp', A2[b][:, :], T["modloc"][b:b + 1, 4 * DS:5 * DS].partition_broadcast(128), r=["modloc"], w=["A2"])
            fw.dma('sp', B2[b][:, :], T["modloc"][b:b + 1, 3 * DS:4 * DS].partition_broadcast(128), r=["modloc"], w=["B2"])
            fw.op('dve', lambda e: e.scalar_tensor_tensor(out=A2[b][:, :], in0=A2[b][:, :], scalar=1.0, in1=nfb[:, :], op0=ALU.add, op1=ALU.mult),
                  r=["A2", "nfb"], w=["A2"])
        fw.op('dve', lambda e: e.tensor_reduce(out=rst[:, :], in_=sa[:, :, :].rearrange("p r t -> p t r"), axis=AX.X, op=ALU.add), r=["sa"], w=["rst"])
        fw.op('dve', lambda e: e.tensor_scalar(out=rst[:, :], in0=rst[:, :], scalar1=1.0 / D, scalar2=EPS, op0=ALU.mult, op1=ALU.add),
              r=["rst"], w=["rst"])
        fw.op('act', lambda e: e.activation(out=rst[:, :], in_=rst[:, :], func=AF.Sqrt), r=["rst"], w=["rst"])
        fw.op('dve', lambda e: e.reciprocal(out=rst[:, :], in_=rst[:, :]), r=["rst"], w=["rst"])
        for tt in range(NTT):
            i2 = tt % 2
            b = (tt * 128) // S
            rows = slice(tt * 128, (tt + 1) * 128)
            fw.dma('sp', x1[i2][:, :], T["x1"][rows, :], r=["x1d"], w=[("x1", i2)])
            fw.op('dve', lambda e: e.scalar_tensor_tensor(out=hf[i2][:, :], in0=x1[i2][:, :], scalar=rst[:, tt:tt + 1], in1=A2[b][:, :],
                                                          op0=ALU.mult, op1=ALU.mult), r=[("x1", i2), "rst", "A2"], w=[("hf", i2)])
            fw.op('pool', lambda e: e.tensor_tensor(out=hf[i2][:, :], in0=hf[i2][:, :], in1=B2[b][:, :], op=ALU.add),
                  r=[("hf", i2), "B2"], w=[("hf", i2)])
            fw.op('act', lambda e: e.activation(out=hb[i2][:, :], in_=hf[i2][:, :], func=AF.Copy), r=[("hf", i2)], w=[("hb", i2)])
            fw.dma('sp', T["hx2_loc"][rows, :], hb[i2][:, :], r=[("hb", i2)], w=["hx2_loc"])
            for cc in range(CD):
                fw.op('pe', lambda e: e.transpose(out=pT[i2][:, cc * 128:(cc + 1) * 128], in_=hf[i2][:, cc * 128:(cc + 1) * 128],
                                                  identity=G["ident_f"]), r=[("hf", i2), "cm_f"], w=[("pT", i2)])
            fw.op('dve', lambda e: e.tensor_copy(out=hTf[i2][:, :, :], in_=pT[i2][:, :CD * 128].rearrange("p (c k) -> p c k", k=128)),
                  r=[("pT", i2)], w=[("hTf", i2)])
            for cc in range(CD):
                fw.op('pe', lambda e: e.matmul(pL[i2][:, :], lhsT=hTf[i2][:, cc, :], rhs=wrf[:, cc, :], start=(cc == 0), stop=(cc == CD - 1)),
                      r=[("hTf", i2), "wrf"], w=[("pL", i2)])
            fw.op('act', lambda e: e.activation(out=lg[i2][:, :], in_=pL[i2][:, :], func=AF.Copy), r=[("pL", i2)], w=[("lg", i2)])
            fw.dma('sp', T["lg_loc"][rows, :], lg[i2][:, :], r=[("lg", i2)], w=["lg_loc"])
        fw.allgather(T["hx2_loc"], T["hx2_all"], r=["hx2_loc"], w=["hx2_all"])
        fw.allgather(T["lg_loc"], T["lg_all"], r=["lg_loc"], w=["lg_all"])
        fw.barrier()


def stage8(fw, c, T, G):
    nc = fw.nc
    DS, CD, KC, NT, NTT, D, S, DE, EC, CAP, NBLK = c.DS, c.CD, c.KC, c.NT, c.NTT, c.D, c.S, c.DE, c.EC, c.CAP, c.NBLK
    BIG = 4 * CAP
    NEG = -1.0e30
    V3 = lambda t, k: t[:, :, :].rearrange("p t (g e) -> p t g e", e=8) if k else t
    pho, sbo, pso = _phase(nc)
    pho.__enter__()
    idi = sbo("s8idi", [128, NTT, 4], I32)
    i1i = sbo("s8i1i", [128, 2, NTT], I32); i2i = sbo("s8i2i", [128, 2, NTT], I32)
    w1 = sbo("s8w1", [128, NTT]); w2 = sbo("s8w2", [128, NTT])
    ph, sb, ps = _phase(nc)
    with ph:
        lg = sb("s8lg", [128, NTT, 36]); tmp = sb("s8tmp", [128, NTT, 36]); brb = sb("s8br", [128, 36])
        gmx = sb("s8gmx", [128, NTT]); gm = sb("s8gm", [128, NTT, 4]); gex = sb("s8gex", [128, NTT, 4]); gpr = sb("s8gpr", [128, NTT])
        es = sb("s8es", [128, NTT, 8]); et = sb("s8et", [128, NTT, 8]); m1 = sb("s8m1", [128, NTT]); mk1 = sb("s8mk1", [128, NTT, 8])
        m2 = sb("s8m2", [128, NTT]); t2 = sb("s8t2", [128, NTT, 8]); w8 = sb("s8w8", [128, NTT, 8]); den = sb("s8den", [128, NTT])
        W32 = sb("s8W", [128, NTT, 32]); M32 = sb("s8M", [128, NTT, 32]); M32b = sb("s8Mb", [128, NTT, 32], BF16)
        pos = sb("s8pos", [128, NTT, 32]); tot = sb("s8tot", [128, NTT, 32]); off = sb("s8off", [128, NTT, 32])
        t32 = sb("s8t32", [128, NTT, 32])
        Mm = sb("s8Mm", [128, NTT, 4]); Pm = sb("s8Pm", [128, NTT, 4]); idf = sb("s8idf", [128, NTT, 4])
        jc = sb("s8jc", [128, 4]); b32 = sb("s8b32", [128, 64])
        i1f = sb("s8i1f", [128, 2, NTT]); i2f = sb("s8i2f", [128, 2, NTT])
        p1 = sb("s8p1", [128, NTT]); p2 = sb("s8p2", [128, NTT])
        pP = ps("s8pP", [128, 512]); pTt = ps("s8pTt", [128, 512])
        bc = lambda t2d, n: t2d[:, :].unsqueeze(2).to_broadcast([128, NTT, n])
        for r in range(8):
            dst = lg if r == 0 else tmp
            fw.dma('sp', dst[:, :, :], T["lg_all"][r * NT:(r + 1) * NT, :].rearrange("(t p) e -> p t e", p=128), r=["lg_all"],
                   w=["lg" if r == 0 else "tmp"])
            if r:
                fw.op('dve', lambda e: e.tensor_tensor(out=lg[:, :, :], in0=lg[:, :, :], in1=tmp[:, :, :], op=ALU.add), r=["lg", "tmp"], w=["lg"])
        fw.dma('sp', brb[:, :], T["br"][0:1, :].partition_broadcast(128), w=["brb"])
        fw.op('dve', lambda e: e.tensor_tensor(out=lg[:, :, :], in0=lg[:, :, :], in1=brb[:, :].unsqueeze(1).to_broadcast([128, NTT, 36]), op=ALU.add),
              r=["lg", "brb"], w=["lg"])
        gl = lg[:, :, 0:4]
        el_ = lg[:, :, 4:36].rearrange("p t (g e) -> p t g e", e=8)
        R = []
        def D_(fn, eng='dve'):
            fw.op(eng, fn, r=["rt"], w=["rt"])
        fw.op('dve', lambda e: e.tensor_reduce(out=gmx[:, :], in_=gl, axis=AX.X, op=ALU.max), r=["lg"], w=["rt"])
        D_(lambda e: e.tensor_tensor(out=gm[:, :, :], in0=gl, in1=bc(gmx, 4), op=ALU.is_ge))
        D_(lambda e: e.tensor_tensor(out=gex[:, :, :], in0=gl, in1=bc(gmx, 4), op=ALU.subtract))
        D_(lambda e: e.activation(out=gex[:, :, :], in_=gex[:, :, :], func=AF.Exp), 'act')
        D_(lambda e: e.tensor_reduce(out=gpr[:, :], in_=gex[:, :, :], axis=AX.X, op=ALU.add))
        D_(lambda e: e.reciprocal(out=gpr[:, :], in_=gpr[:, :]))
        for g in range(4):
            dst = es if g == 0 else et
            D_(lambda e: e.tensor_tensor(out=dst[:, :, :], in0=el_[:, :, g, :], in1=gm[:, :, g:g + 1].to_broadcast([128, NTT, 8]), op=ALU.mult))
            if g:
                D_(lambda e: e.tensor_tensor(out=es[:, :, :], in0=es[:, :, :], in1=et[:, :, :], op=ALU.add))
        D_(lambda e: e.tensor_reduce(out=m1[:, :], in_=es[:, :, :], axis=AX.X, op=ALU.max))
        D_(lambda e: e.tensor_tensor(out=mk1[:, :, :], in0=es[:, :, :], in1=bc(m1, 8), op=ALU.is_ge))
        D_(lambda e: e.scalar_tensor_tensor(out=et[:, :, :], in0=mk1[:, :, :], scalar=NEG, in1=es[:, :, :], op0=ALU.mult, op1=ALU.add))
        D_(lambda e: e.tensor_reduce(out=m2[:, :], in_=et[:, :, :], axis=AX.X, op=ALU.max))
        D_(lambda e: e.tensor_tensor(out=t2[:, :, :], in0=es[:, :, :], in1=bc(m2, 8), op=ALU.is_ge))
        D_(lambda e: e.tensor_tensor(out=w8[:, :, :], in0=es[:, :, :], in1=bc(m1, 8), op=ALU.subtract))
        D_(lambda e: e.activation(out=w8[:, :, :], in_=w8[:, :, :], func=AF.Exp), 'act')
        D_(lambda e: e.tensor_tensor(out=w8[:, :, :], in0=w8[:, :, :], in1=t2[:, :, :], op=ALU.mult))
        D_(lambda e: e.tensor_reduce(out=den[:, :], in_=w8[:, :, :], axis=AX.X, op=ALU.add))
        D_(lambda e: e.reciprocal(out=den[:, :], in_=den[:, :]))
        D_(lambda e: e.tensor_tensor(out=den[:, :], in0=den[:, :], in1=gpr[:, :], op=ALU.mult))
        D_(lambda e: e.tensor_tensor(out=w8[:, :, :], in0=w8[:, :, :], in1=bc(den, 8), op=ALU.mult))
        W4 = W32[:, :, :].rearrange("p t (g e) -> p t g e", e=8)
        for g in range(4):
            D_(lambda e: e.tensor_tensor(out=W4[:, :, g, :], in0=w8[:, :, :], in1=gm[:, :, g:g + 1].to_broadcast([128, NTT, 8]), op=ALU.mult))
        D_(lambda e: e.tensor_scalar(out=M32[:, :, :], in0=W32[:, :, :], scalar1=0.0, scalar2=None, op0=ALU.is_gt))
        D_(lambda e: e.activation(out=M32b[:, :, :], in_=M32[:, :, :], func=AF.Copy), 'act')
        NCOL = NTT * 32
        Mf = M32b[:, :, :].rearrange("p t e -> p (t e)")
        posf = pos[:, :, :].rearrange("p t e -> p (t e)")
        totf = tot[:, :, :].rearrange("p t e -> p (t e)")
        for c0 in range(0, NCOL, 512):
            n = min(512, NCOL - c0)
            fw.op('pe', lambda e: e.matmul(pP[:, :n], lhsT=G["triexc_b"], rhs=Mf[:, c0:c0 + n], start=True, stop=True), r=["rt", "cm_b"], w=["pP"])
            fw.op('dve', lambda e: e.tensor_copy(out=posf[:, c0:c0 + n], in_=pP[:, :n]), r=["pP"], w=["rt"])
            fw.op('pe', lambda e: e.matmul(pTt[:, :n], lhsT=G["ones_b"], rhs=Mf[:, c0:c0 + n], start=True, stop=True), r=["rt", "cm_b"], w=["pTt"])
            fw.op('dve', lambda e: e.tensor_copy(out=totf[:, c0:c0 + n], in_=pTt[:, :n]), r=["pTt"], w=["rt"])
        D_(lambda e: e.memset(off[:, 0, :], 0.0))
        for t in range(1, NTT):
            D_(lambda e: e.tensor_tensor(out=off[:, t, :], in0=off[:, t - 1, :], in1=tot[:, t - 1, :], op=ALU.add))
        D_(lambda e: e.tensor_tensor(out=pos[:, :, :], in0=pos[:, :, :], in1=off[:, :, :], op=ALU.add))
        mine = G["rk"][:, 8:40].unsqueeze(1).to_broadcast([128, NTT, 32])
        fw.op('dve', lambda e: e.tensor_tensor(out=t32[:, :, :], in0=M32[:, :, :], in1=mine, op=ALU.mult), r=["rt", "rk"], w=["rt"])
        D_(lambda e: e.tensor_reduce(out=Mm[:, :, :], in_=t32[:, :, :].rearrange("p t (g e) -> p t g e", e=8), axis=AX.X, op=ALU.add))
        fw.op('dve', lambda e: e.tensor_tensor(out=t32[:, :, :], in0=pos[:, :, :], in1=mine, op=ALU.mult), r=["rt", "rk"], w=["rt"])
        D_(lambda e: e.tensor_reduce(out=Pm[:, :, :], in_=t32[:, :, :].rearrange("p t (g e) -> p t g e", e=8), axis=AX.X, op=ALU.add))
        fw.op('dve', lambda e: e.tensor_copy(out=jc[:, :], in_=G["rk"][:, 4:8]), r=["rk"], w=["rt"])
        D_(lambda e: e.tensor_scalar(out=idf[:, :, :], in0=Pm[:, :, :], scalar1=float(CAP), scalar2=None, op0=ALU.is_lt))
        D_(lambda e: e.tensor_tensor(out=idf[:, :, :], in0=idf[:, :, :], in1=Mm[:, :, :], op=ALU.mult))
        D_(lambda e: e.tensor_tensor(out=Pm[:, :, :], in0=Pm[:, :, :], in1=jc[:, :].unsqueeze(1).to_broadcast([128, NTT, 4]), op=ALU.add))
        D_(lambda e: e.tensor_tensor(out=idf[:, :, :], in0=idf[:, :, :], in1=Pm[:, :, :], op=ALU.mult))
        D_(lambda e: e.tensor_scalar(out=idf[:, :, :], in0=idf[:, :, :], scalar1=float(BIG), scalar2=None, op0=ALU.add))
        D_(lambda e: e.tensor_copy(out=idi[:, :, :], in_=idf[:, :, :]))
        fw.op('dve', lambda e: e.tensor_copy(out=b32[:, :], in_=G["rkb"][:, :]), r=["rkb"], w=["rt"])
        D_(lambda e: e.tensor_tensor(out=t2[:, :, :], in0=t2[:, :, :], in1=mk1[:, :, :], op=ALU.subtract))
        O4 = off[:, :, :].rearrange("p t (g e) -> p t g e", e=8)
        for (mk, idx_o, w_o, p_o) in ((mk1, i1f, w1, p1), (t2, i2f, w2, p2)):
            for g in range(4):
                D_(lambda e: e.tensor_tensor(out=O4[:, :, g, :], in0=mk[:, :, :], in1=gm[:, :, g:g + 1].to_broadcast([128, NTT, 8]), op=ALU.mult))
            D_(lambda e: e.tensor_tensor(out=t32[:, :, :], in0=off[:, :, :], in1=W32[:, :, :], op=ALU.mult))
            D_(lambda e: e.tensor_reduce(out=w_o[:, :], in_=t32[:, :, :], axis=AX.X, op=ALU.add))
            D_(lambda e: e.tensor_tensor(out=t32[:, :, :], in0=off[:, :, :], in1=pos[:, :, :], op=ALU.mult))
            D_(lambda e: e.tensor_reduce(out=p_o[:, :], in_=t32[:, :, :], axis=AX.X, op=ALU.add))
            D_(lambda e: e.tensor_scalar(out=p_o[:, :], in0=p_o[:, :], scalar1=float(CAP), scalar2=None, op0=ALU.is_lt))
            D_(lambda e: e.tensor_tensor(out=w_o[:, :], in0=w_o[:, :], in1=p_o[:, :], op=ALU.mult))
            for ab in range(2):
                D_(lambda e: e.tensor_tensor(out=tot[:, :, :], in0=pos[:, :, :],
                                             in1=b32[:, ab * 32:(ab + 1) * 32].unsqueeze(1).to_broadcast([128, NTT, 32]), op=ALU.add))
                D_(lambda e: e.tensor_tensor(out=t32[:, :, :], in0=off[:, :, :], in1=tot[:, :, :], op=ALU.mult))
                D_(lambda e: e.tensor_reduce(out=idx_o[:, ab, :], in_=t32[:, :, :], axis=AX.X, op=ALU.add))
                D_(lambda e: e.tensor_tensor(out=idx_o[:, ab, :], in0=idx_o[:, ab, :], in1=p_o[:, :], op=ALU.mult))
        D_(lambda e: e.tensor_copy(out=i1i[:, :, :], in_=i1f[:, :, :]))
        D_(lambda e: e.tensor_copy(out=i2i[:, :, :], in_=i2f[:, :, :]))

        fw.barrier()
    ph, sb, ps = _phase(nc)
    with ph:
        zt = sb("s8zt", [128, D], BF16)
        hr = [sb("s8hr%d" % i, [128, 8, DS], BF16) for i in range(2)]
        fw.op('pool', lambda e: e.memset(zt[:, :], 0.0), w=["zt"])
        for r0 in range(0, 4 * CAP, 128):
            fw.dma('sp', T["Xsel"][r0:r0 + 128, :], zt[:, :], r=["zt"], w=[("Xz", r0)])
        reg_sc = nc.gpsimd.alloc_register("bc_sc"); nc.gpsimd.reg_mov(reg_sc, 4 * CAP - 1)
        reg_ga = nc.gpsimd.alloc_register("bc_ga"); nc.gpsimd.reg_mov(reg_ga, 64 * 2 * CAP - 1)
        hv = T["hx2_all"][:, :].rearrange("(r n) d -> n r d", r=8)
        for tt in range(NTT):
            i2 = tt % 2
            fw.dma('sp', hr[i2][:, :, :], hv[tt * 128:(tt + 1) * 128, :, :], r=["hx2_all"], w=[("hr", i2)])
            for j in range(4):
                fw.idma(out=T["Xsel"][:, :], out_offset=bass.IndirectOffsetOnAxis(ap=idi[:, tt, j:j + 1], axis=0),
                        in_=hr[i2][:, :, :].rearrange("p r d -> p (r d)"), in_offset=None, bounds_check=reg_sc, oob_is_err=False,
                        r=[("hr", i2)] + [("Xz", r0_) for r0_ in range(0, 4 * CAP, 128)], w=[("Xsc", tt, j)])

        HB = min(512, DE)
        NH = DE // HB
        QH = HB // 128
        wb = sb("s8wb", [128, max(2 * KC * HB, EC * D)], BF16)
        wg = wb[:, 0:KC * HB].rearrange("p (k n) -> p k n", n=HB)
        wu = wb[:, KC * HB:2 * KC * HB].rearrange("p (k n) -> p k n", n=HB)
        wd = wb[:, 0:EC * D].rearrange("p (k n) -> p k n", n=D)
        xs = [sb("s8xs%d" % i, [128, D], BF16) for i in range(2)]
        xT = [sb("s8xT%d" % i, [128, KC, 128], BF16) for i in range(2)]
        sa_ = [sb("s8sa%d" % i, [128, HB]) for i in range(2)]; hs = [sb("s8hs%d" % i, [128, HB], BF16) for i in range(2)]
        hTa = sb("s8hTa", [128, EC, CAP], BF16)
        yo = [sb("s8yo%d" % i, [128, D], BF16) for i in range(2)]
        pX = ps("s8pX", [128, 1024], BF16); pG = ps("s8pG", [128, 512]); pUu = ps("s8pUu", [128, 512])
        pH = ps("s8pH", [128, 1024], BF16); pYy = [ps("s8pY%d" % i, [128, 512]) for i in range(2)]
        it = 0
        yvs = [T["Yloc_" + ab][:, :].rearrange("(cb s) d -> s cb d", cb=8) for ab in "ab"]
        for j in range(4):
            for hh in range(NH):
                for kc in range(KC):
                    fw.dma('pool', wg[:, kc, :], T["weg"][j, kc * 128:(kc + 1) * 128, hh * HB:(hh + 1) * HB], w=["wb"])
                    fw.dma('pool', wu[:, kc, :], T["weu"][j, kc * 128:(kc + 1) * 128, hh * HB:(hh + 1) * HB], w=["wb"])
                for sbk in range(NBLK):
                    i2 = it % 2; it += 1
                    r0 = j * CAP + sbk * 128
                    fw.dma('sp', xs[i2][:, :], T["Xsel"][r0:r0 + 128, :], r=[("Xsc", tt_, j_) for tt_ in range(NTT) for j_ in range(4)], w=[("xs", i2)])
                    for k0 in range(0, KC, 8):
                        for kc in range(k0, k0 + 8):
                            fw.op('pe', lambda e: e.transpose(out=pX[:, (kc - k0) * 128:(kc - k0 + 1) * 128], in_=xs[i2][:, kc * 128:(kc + 1) * 128],
                                                              identity=G["ident_b"]), r=[("xs", i2), "cm_b"], w=["pX"])
                        fw.op('act' if (k0 // 8) % 2 == 0 else 'dve',
                              (lambda e: e.activation(out=xT[i2][:, k0:k0 + 8, :], in_=pX[:, :].rearrange("p (k n) -> p k n", n=128), func=AF.Copy))
                              if (k0 // 8) % 2 == 0 else
                              (lambda e: e.tensor_copy(out=xT[i2][:, k0:k0 + 8, :], in_=pX[:, :].rearrange("p (k n) -> p k n", n=128))),
                              r=["pX"], w=[("xT", i2)])
                    for kc in range(KC):
                        fw.op('pe', lambda e: e.matmul(pG[:, :HB], lhsT=xT[i2][:, kc, :], rhs=wg[:, kc, :], start=(kc == 0), stop=(kc == KC - 1)),
                              r=[("xT", i2), "wb"], w=["pG"])
                    for kc in range(KC):
                        fw.op('pe', lambda e: e.matmul(pUu[:, :HB], lhsT=xT[i2][:, kc, :], rhs=wu[:, kc, :], start=(kc == 0), stop=(kc == KC - 1)),
                              r=[("xT", i2), "wb"], w=["pUu"])
                    fw.op('act', lambda e: e.activation(out=sa_[i2][:, :], in_=pG[:, :HB], func=AF.Silu), r=["pG"], w=[("sa", i2)])
                    fw.op('dve', lambda e: e.tensor_tensor(out=hs[i2][:, :], in0=sa_[i2][:, :], in1=pUu[:, :HB], op=ALU.mult),
                          r=[("sa", i2), "pUu"], w=[("hs", i2)])
                    for q in range(QH):
                        fw.op('pe', lambda e: e.transpose(out=pH[:, q * 128:(q + 1) * 128], in_=hs[i2][:, q * 128:(q + 1) * 128],
                                                          identity=G["ident_b"]), r=[("hs", i2), "cm_b"], w=["pH"])
                    fw.op('act', lambda e: e.activation(out=hTa[:, hh * QH:(hh + 1) * QH, sbk * 128:(sbk + 1) * 128],
                                                        in_=pH[:, :QH * 128].rearrange("p (q n) -> p q n", n=128), func=AF.Copy),
                          r=["pH"], w=["hTa"])
            for ec in range(EC):
                fw.dma('pool', wd[:, ec, :], T["wed"][j, ec * 128:(ec + 1) * 128, :], r=[], w=["wb"])
            for sbk in range(NBLK):
                i2 = it % 2; it += 1
                for nb in range(D // 512):
                    pi = nb % 2
                    for ec in range(EC):
                        fw.op('pe', lambda e: e.matmul(pYy[pi][:, :], lhsT=hTa[:, ec, sbk * 128:(sbk + 1) * 128], rhs=wd[:, ec, nb * 512:(nb + 1) * 512],
                                                       start=(ec == 0), stop=(ec == EC - 1)), r=["hTa", "wb"], w=[("pY", pi)])
                    if pi == 0:
                        fw.op('act', lambda e: e.activation(out=yo[i2][:, nb * 512:(nb + 1) * 512], in_=pYy[pi][:, :], func=AF.Copy),
                              r=[("pY", pi)], w=[("yo", i2)])
                    else:
                        fw.op('dve', lambda e: e.tensor_copy(out=yo[i2][:, nb * 512:(nb + 1) * 512], in_=pYy[pi][:, :]), r=[("pY", pi)], w=[("yo", i2)])
                r0 = (j % 2) * CAP + sbk * 128
                fw.dma('sp', yvs[j // 2][r0:r0 + 128, :, :], yo[i2][:, :].rearrange("p (cb d) -> p cb d", cb=8), r=[("yo", i2)], w=["Yloc"])
            if j == 1:
                fw.allgather(T["Yloc_a"], T["Yall_a"], r=["Yloc"], w=["Yall_a"])
        fw.allgather(T["Yloc_b"], T["Yall_b"], r=["Yloc"], w=["Yall_b"])

        fw.barrier()
    ph, sb, ps = _phase(nc)
    with ph:
        y1 = [sb("s8y1%d" % i, [128, 2, DS], BF16) for i in range(2)]; y2 = [sb("s8y2%d" % i, [128, 2, DS], BF16) for i in range(2)]
        x1 = [sb("s8x1%d" % i, [128, DS]) for i in range(2)]; ac = [sb("s8ac%d" % i, [128, DS]) for i in range(2)]
        gtf = [sb("s8gtf%d" % i, [128, DS]) for i in range(2)]
        for b in range(2):
            fw.dma('sp', gtf[b][:, :], T["modloc"][b:b + 1, 5 * DS:6 * DS].partition_broadcast(128), r=["modloc"], w=["gtf"])
        NR = 64 * 4 * CAP
        for tt in range(NTT):
            i2 = tt % 2
            b = (tt * 128) // S
            rows = slice(tt * 128, (tt + 1) * 128)
            for ab in range(2):
                src = T["Yall_" + "ab"[ab]][:, :]
                fw.op('pool', lambda e: e.memset(y1[i2][:, ab, :], 0.0), w=[("y1", i2, ab)])
                fw.op('pool', lambda e: e.memset(y2[i2][:, ab, :], 0.0), w=[("y2", i2, ab)])
                fw.idma(out=y1[i2][:, ab, :], out_offset=None, in_=src, in_offset=bass.IndirectOffsetOnAxis(ap=i1i[:, ab, tt:tt + 1], axis=0),
                        bounds_check=reg_ga, oob_is_err=False, r=["Yall_a", "Yall_b"], w=[("y1", i2, ab)])
                fw.idma(out=y2[i2][:, ab, :], out_offset=None, in_=src, in_offset=bass.IndirectOffsetOnAxis(ap=i2i[:, ab, tt:tt + 1], axis=0),
                        bounds_check=reg_ga, oob_is_err=False, r=["Yall_a", "Yall_b"], w=[("y2", i2, ab)])
            fw.op('pool', lambda e: e.tensor_tensor(out=y1[i2][:, 0, :], in0=y1[i2][:, 0, :], in1=y1[i2][:, 1, :], op=ALU.add),
                  r=[("y1", i2, 0), ("y1", i2, 1)], w=[("y1", i2, 0)])
            fw.op('pool', lambda e: e.tensor_tensor(out=y2[i2][:, 0, :], in0=y2[i2][:, 0, :], in1=y2[i2][:, 1, :], op=ALU.add),
                  r=[("y2", i2, 0), ("y2", i2, 1)], w=[("y2", i2, 0)])
            fw.dma('sp', x1[i2][:, :], T["x1"][rows, :], r=["x1d"], w=[("x1", i2)])
            fw.op('dve', lambda e: e.tensor_scalar(out=ac[i2][:, :], in0=y1[i2][:, 0, :], scalar1=w1[:, tt:tt + 1], scalar2=None, op0=ALU.mult),
                  r=[("y1", i2, 0), "rt"], w=[("ac", i2)])
            fw.op('dve', lambda e: e.scalar_tensor_tensor(out=ac[i2][:, :], in0=y2[i2][:, 0, :], scalar=w2[:, tt:tt + 1], in1=ac[i2][:, :],
                                                          op0=ALU.mult, op1=ALU.add), r=[("y2", i2, 0), ("ac", i2), "rt"], w=[("ac", i2)])
            fw.op('pool', lambda e: e.tensor_tensor(out=ac[i2][:, :], in0=ac[i2][:, :], in1=gtf[b][:, :], op=ALU.mult), r=[("ac", i2), "gtf"], w=[("ac", i2)])
            fw.op('dve', lambda e: e.tensor_tensor(out=ac[i2][:, :], in0=ac[i2][:, :], in1=x1[i2][:, :], op=ALU.add), r=[("ac", i2), ("x1", i2)], w=[("ac", i2)])
            fw.dma('sp', T["out"][rows, :], ac[i2][:, :], r=[("ac", i2)], w=["out"])
        fw.barrier()
    pho.__exit__(None, None, None)


def T_rkb(G):
    return G["rkb"][:, :]


def _consts(c):
    k = np.arange(128)[:, None]
    m = np.arange(128)[None, :]
    ident = (k == m)
    triinc = (k <= m)
    tridec = (k >= m)
    ones = np.ones((128, 128), bool)
    partner = np.where((np.arange(128) % 64) < 32, np.arange(128) + 32, np.arange(128) - 32)
    perm = (k == partner[None, :])
    triexc = (k < m)
    cm = np.concatenate([ident, triinc, tridec, ones, perm, triexc], axis=1).astype(np.float32)
    S, GW = c.S, c.GW
    s = np.arange(S)
    row = (s // GW).astype(np.float32)
    col = (s % GW).astype(np.float32)
    inv = (ROPE_THETA ** (-np.arange(32, dtype=np.float32) / 32.0)).astype(np.float32)
    d = np.arange(128)
    pos = np.where((d < 64)[:, None], row[None, :], col[None, :]).astype(np.float32)
    ang = (pos * inv[d % 32][:, None]).astype(np.float32)
    sign = np.where((d % 64) < 32, -1.0, 1.0).astype(np.float32)[:, None]
    return cm, np.cos(ang).astype(np.float32), (np.sin(ang) * sign).astype(np.float32)


def prep(c, inp):
    D, S, CTX, DS, KC, HPG, HQ, QF, NT = c.D, c.S, c.CTX, c.DS, c.KC, c.HPG, c.HQ, c.QF, c.NT
    f = lambda a: np.ascontiguousarray(np.asarray(a, dtype=np.float32))
    x = f(inp["x"]).reshape(NT, D)
    ctx = f(inp["ctx"]).reshape(2 * CTX, D)
    cvec = np.concatenate([f(inp["c"]), f(inp["c_ctx"])[None, :]], axis=0)
    w_ada = f(inp["w_ada"])[0]; b_ada = f(inp["b_ada"])[0]
    w_in = f(inp["w_in"])[0]
    KVD = 8 * 128; BC = 8 * 128; XBC = D + 2 * BC; SH = D // 64
    ok_, ov_, ox_ = 0, KVD, 2 * KVD
    oB_, oC_ = ox_ + D, ox_ + D + BC
    odf_ = 2 * KVD + XBC; odb_ = odf_ + SH
    oq_ = odb_ + SH; oga_ = oq_ + 8 * QF; ogs_ = oga_ + D; oz_ = ogs_ + D
    conv_w = f(inp["conv_w"])[0]; conv_b = f(inp["conv_b"])[0]
    cm, rc, rs = _consts(c)
    maps = []
    for g in range(8):
        m = {}
        m["xtok"] = f(x[g * c.TPC:(g + 1) * c.TPC]); m["ctok"] = f(ctx[g * c.CT:(g + 1) * c.CT])
        m["xcol"] = f(x[:, g * DS:(g + 1) * DS]); m["cvec"] = cvec
        cols = np.concatenate([np.arange(j * D + g * DS, j * D + (g + 1) * DS) for j in range(6)])
        m["wada"] = f(w_ada[:, cols]); m["bada"] = f(b_ada[cols][None, :])
        m["nmw"] = f(f(inp["norm_mix_w"])[0].reshape(KC, 128).T)
        m["nfw"] = f(f(inp["norm_ffn_w"])[0][g * DS:(g + 1) * DS][None, :])
        ar = np.arange
        cols = np.concatenate([
            oq_ + g * QF + ar(QF), ok_ + g * 128 + ar(128), ov_ + g * 128 + ar(128), ox_ + g * DS + ar(DS),
            oB_ + g * 128 + ar(128), oC_ + g * 128 + ar(128), odf_ + g * HPG + ar(HPG), odb_ + g * HPG + ar(HPG),
            oga_ + g * DS + ar(DS), ogs_ + g * DS + ar(DS), oz_ + g * DS + ar(DS)])
        assert len(cols) == c.NC1
        m["w1"] = f(w_in[:, cols])
        m["qknw"] = f(np.stack([f(inp["q_norm_w"])[0], f(inp["k_norm_w"])[0]], axis=1))
        ch = np.concatenate([g * DS + ar(DS), D + g * 128 + ar(128), D + BC + g * 128 + ar(128)])
        cw = np.concatenate([conv_w[:, ch].T, conv_b[ch][:, None]], axis=1)
        m["convw"] = f(cw.reshape(c.NCC, 128, 6).transpose(1, 0, 2).reshape(128, c.NCC * 6))
        hs = slice(g * HPG, (g + 1) * HPG)
        m["ssdv"] = f(np.concatenate([f(inp[k])[0][hs] for k in ("a_log_f", "a_log_b", "dt_bias_f", "dt_bias_b", "d_skip")])[None, :])
        m["snw"] = f(f(inp["ssd_norm_w"])[0][g * DS:(g + 1) * DS][None, :])
        m["wap"] = f(f(inp["w_attn_proj"])[0][:, g * DS:(g + 1) * DS])
        m["wsp"] = f(f(inp["w_ssd_proj"])[0][:, g * DS:(g + 1) * DS])
        m["wout"] = f(f(inp["w_out"])[0][:, g * DS:(g + 1) * DS])
        wr = np.concatenate([f(inp["w_router_group"])[0], f(inp["w_router_expert"])[0]], axis=1)
        m["wr"] = f(wr[g * DS:(g + 1) * DS]); m["br"] = f(np.concatenate([f(inp["b_router_group"])[0], f(inp["b_router_expert"])[0]])[None, :])
        ex = [j * 8 + g for j in range(4)]
        m["weg"] = f(f(inp["w_exp_gate"])[0][ex]); m["weu"] = f(f(inp["w_exp_up"])[0][ex]); m["wed"] = f(f(inp["w_exp_down"])[0][ex])
        m["cmat"] = cm; m["ropec"] = rc; m["ropes"] = rs
        rk = np.zeros((128, 40), np.float32)
        rk[:, 0] = g; rk[:, 1] = float(g >= 4)
        rk[:, 8:40] = ((np.arange(32) % 8) == g).astype(np.float32)[None, :]
        rk[:, 4:8] = (np.arange(4) * c.CAP - 4 * c.CAP).astype(np.float32)[None, :]
        m["rk"] = rk
        je = np.arange(32)
        base = (((je % 8) * 8 + g) * 2 * c.CAP + ((je // 8) % 2) * c.CAP).astype(np.float32)
        OOB = np.float32(64 * 2 * c.CAP)
        ba = np.where(je // 8 < 2, base, OOB); bb = np.where(je // 8 >= 2, base, OOB)
        m["rkb"] = np.broadcast_to(np.concatenate([ba, bb])[None, :].astype(np.float32), (128, 64)).copy()
        maps.append(m)
    return maps


def run(c, inputs, debug=()):
    nc = build(c, debug=debug)
    maps = prep(c, inputs)
    res = run_bass_kernel_spmd(nc, maps, core_ids=list(range(8)))
    out = np.concatenate([np.asarray(r["out"]) for r in res.results], axis=1)
    return out.reshape(2, c.S, c.D).astype(np.float32), res.results


def kernel(**inputs):
    c = Cfg()
    out, _ = run(c, inputs)
    return out
```
